# Optimizing a Trainium2 kernel written in Bass

```python
import math
import jax, jax.numpy as jnp
from jax import lax
import numpy as np

D_MODEL = 2048
BATCH = 8
SEQ = 2048
DEPTH = 1

N_META = 16
EPS = 1e-6
HEAD_DIM = 64
N_Q_HEADS = 16
N_KV_HEADS = 2
GROUP = N_Q_HEADS // N_KV_HEADS
WINDOW = 128
BLOCK = 128
Q_DIM = N_Q_HEADS * HEAD_DIM
KV_DIM = N_KV_HEADS * HEAD_DIM
LRU_WIDTH = 1024
LRU_BLOCKS = 16
LRU_BW = LRU_WIDTH // LRU_BLOCKS
CONV_W = 4
LRU_C = 8.0
IN_DIM = Q_DIM + 2 * KV_DIM + 2 * LRU_WIDTH
MIX_DIM = Q_DIM + LRU_WIDTH
PEER_HEADS = 8
N_KEYS = 128
N_EXPERTS = N_KEYS * N_KEYS
PEER_TOPK = 16
PEER_DK = 256
PEER_DK_HALF = PEER_DK // 2
PEER_CHUNK = 128

kernel_name = "hymba_swa_rglru_peer_block"


def rmsnorm(x, g):
    xf = x.astype(jnp.float32)
    y = xf * lax.rsqrt(jnp.mean(xf * xf, axis=-1, keepdims=True) + EPS)
    return (y * g.astype(jnp.float32)).astype(x.dtype)


def sink_softmax(s, mask, sink_b):
    s = jnp.where(mask, s, -1e30)
    sink = jnp.broadcast_to(sink_b, s.shape[:-1] + (1,))
    p = jax.nn.softmax(jnp.concatenate([s, sink], axis=-1), axis=-1)
    return p[..., :-1]


def window_attention(q, k, v, sinks):
    B, T, _ = q.shape
    S = T - N_META
    nb = S // BLOCK
    scale = HEAD_DIM ** -0.5
    q = q.reshape(B, T, N_KV_HEADS, GROUP, HEAD_DIM)
    k = k.reshape(B, T, N_KV_HEADS, HEAD_DIM)
    v = v.reshape(B, T, N_KV_HEADS, HEAD_DIM)
    sink_hg = sinks.astype(jnp.float32).reshape(N_KV_HEADS, GROUP)
    qm, qr = q[:, :N_META], q[:, N_META:]
    km, kr = k[:, :N_META], k[:, N_META:]
    vm, vr = v[:, :N_META], v[:, N_META:]

    s_m = jnp.einsum('bqhgd,bkhd->bhgqk', qm, km).astype(jnp.float32) * scale
    mask_m = jnp.tril(jnp.ones((N_META, N_META), bool))
    p_m = sink_softmax(s_m, mask_m, sink_hg[None, :, :, None, None])
    o_m = jnp.einsum('bhgqk,bkhd->bqhgd', p_m.astype(v.dtype), vm).reshape(B, N_META, Q_DIM)

    qb = qr.reshape(B, nb, BLOCK, N_KV_HEADS, GROUP, HEAD_DIM)
    kp = jnp.pad(kr, ((0, 0), (BLOCK, 0), (0, 0), (0, 0))).reshape(B, nb + 1, BLOCK, N_KV_HEADS, HEAD_DIM)
    vp = jnp.pad(vr, ((0, 0), (BLOCK, 0), (0, 0), (0, 0))).reshape(B, nb + 1, BLOCK, N_KV_HEADS, HEAD_DIM)
    k_meta = jnp.broadcast_to(km[:, None], (B, nb, N_META, N_KV_HEADS, HEAD_DIM))
    v_meta = jnp.broadcast_to(vm[:, None], (B, nb, N_META, N_KV_HEADS, HEAD_DIM))
    k_all = jnp.concatenate([k_meta, kp[:, :-1], kp[:, 1:]], axis=2)
    v_all = jnp.concatenate([v_meta, vp[:, :-1], vp[:, 1:]], axis=2)
    s_r = jnp.einsum('bnqhgd,bnkhd->bnhgqk', qb, k_all).astype(jnp.float32) * scale
    q_coord = jnp.arange(BLOCK)[:, None] + BLOCK
    k_coord = jnp.arange(2 * BLOCK)[None, :]
    dist = q_coord - k_coord
    band = (dist >= 0) & (dist < WINDOW)
    valid = (jnp.arange(nb)[:, None] > 0) | (k_coord >= BLOCK)
    mask_r = jnp.concatenate([jnp.ones((nb, BLOCK, N_META), bool),
                              band[None] & valid[:, None, :]], axis=-1)
    p_r = sink_softmax(s_r, mask_r[None, :, None, None], sink_hg[None, None, :, :, None, None])
    o_r = jnp.einsum('bnhgqk,bnkhd->bnqhgd', p_r.astype(v.dtype), v_all).reshape(B, S, Q_DIM)
    return jnp.concatenate([o_m, o_r], axis=1)


def rglru_branch(xb, gate, conv_w, conv_b, w_r, b_r, w_i, b_i, lam):
    B, T, W = xb.shape
    xp = jnp.pad(xb, ((0, 0), (CONV_W - 1, 0), (0, 0)))
    xc = conv_b
    for tap in range(CONV_W):
        xc = xc + conv_w[tap] * xp[:, tap:tap + T]
    xh = xc.reshape(B, T, LRU_BLOCKS, LRU_BW)
    r = jax.nn.sigmoid(jnp.einsum('btnc,ncd->btnd', xh, w_r) + b_r).reshape(B, T, W)
    i = jax.nn.sigmoid(jnp.einsum('btnc,ncd->btnd', xh, w_i) + b_i).reshape(B, T, W)
    log_a = LRU_C * r.astype(jnp.float32) * jax.nn.log_sigmoid(lam.astype(jnp.float32))
    a = jnp.exp(log_a)
    mult = jnp.sqrt(-jnp.expm1(2.0 * log_a))
    bx = mult * (i * xc).astype(jnp.float32)

    def combine(left, right):
        a_l, b_l = left
        a_r, b_r_ = right
        return a_l * a_r, a_r * b_l + b_r_

    _, h = lax.associative_scan(combine, (a, bx), axis=1)
    return (jax.nn.gelu(gate) * h.astype(xb.dtype))


def peer(x2d, w_q, sub_keys, u_tab, v_tab):
    n_tok, D = x2d.shape
    pad = (-n_tok) % PEER_CHUNK
    xp = jnp.pad(x2d, ((0, pad), (0, 0))).reshape(-1, PEER_CHUNK, D)

    def one_block(xc):
        q = (xc @ w_q).reshape(PEER_CHUNK, PEER_HEADS, 2, PEER_DK_HALF)
        s = jnp.einsum('chpd,hpkd->chpk', q, sub_keys).astype(jnp.float32)
        top_s, top_i = lax.top_k(s, PEER_TOPK)
        cand = top_s[:, :, 0, :, None] + top_s[:, :, 1, None, :]
        best_s, best_c = lax.top_k(cand.reshape(PEER_CHUNK, PEER_HEADS, PEER_TOPK * PEER_TOPK), PEER_TOPK)
        i1 = jnp.take_along_axis(top_i[:, :, 0], best_c // PEER_TOPK, axis=-1)
        i2 = jnp.take_along_axis(top_i[:, :, 1], best_c % PEER_TOPK, axis=-1)
        experts = i1 * N_KEYS + i2
        g = jax.nn.softmax(best_s, axis=-1)
        u = u_tab[experts]
        act = jax.nn.gelu(jnp.einsum('chkd,cd->chk', u, xc))
        v = v_tab[experts]
        return jnp.einsum('chk,chkd->cd', (g * act.astype(jnp.float32)).astype(v.dtype), v)

    out = lax.map(one_block, xp)
    return out.reshape(-1, D)[:n_tok]


def setup_inputs(seed: int = 0) -> dict:
    key = jax.random.key(seed)
    ks = jax.random.split(key, 24)
    f32 = jnp.float32

    def nrm(k, shape, scale):
        return jax.random.normal(k, shape, f32) * scale

    def gain(k, shape):
        return 1.0 + 0.02 * jax.random.normal(k, shape, f32)

    u_a = jax.random.uniform(ks[10], (DEPTH, LRU_WIDTH), f32, 0.9, 0.999)
    sig_l = u_a ** (1.0 / LRU_C)
    lru_lambda = jnp.log(sig_l) - jnp.log1p(-sig_l)
    return {
        "x": jax.random.normal(ks[0], (BATCH, SEQ, D_MODEL), f32),
        "meta_tokens": nrm(ks[1], (N_META, D_MODEL), 1.0),
        "norm_mix_g": gain(ks[2], (DEPTH, D_MODEL)),
        "w_in": nrm(ks[3], (DEPTH, D_MODEL, IN_DIM), D_MODEL ** -0.5),
        "b_in": nrm(ks[4], (DEPTH, IN_DIM), 0.02),
        "sinks": nrm(ks[5], (DEPTH, N_Q_HEADS), 0.5),
        "conv_w": nrm(ks[6], (DEPTH, CONV_W, LRU_WIDTH), CONV_W ** -0.5),
        "conv_b": nrm(ks[7], (DEPTH, LRU_WIDTH), 0.02),
        "w_r": nrm(ks[8], (DEPTH, LRU_BLOCKS, LRU_BW, LRU_BW), LRU_BW ** -0.5),
        "b_r": nrm(ks[9], (DEPTH, LRU_BLOCKS, LRU_BW), 0.02),
        "w_i": nrm(ks[11], (DEPTH, LRU_BLOCKS, LRU_BW, LRU_BW), LRU_BW ** -0.5),
        "b_i": nrm(ks[12], (DEPTH, LRU_BLOCKS, LRU_BW), 0.02),
        "lru_lambda": lru_lambda,
        "gn_attn_g": gain(ks[13], (DEPTH, Q_DIM)),
        "gn_lru_g": gain(ks[14], (DEPTH, LRU_WIDTH)),
        "w_out": nrm(ks[15], (DEPTH, MIX_DIM, D_MODEL), MIX_DIM ** -0.5),
        "norm_ffn_g": gain(ks[16], (DEPTH, D_MODEL)),
        "peer_wq": nrm(ks[17], (DEPTH, D_MODEL, PEER_HEADS * PEER_DK), D_MODEL ** -0.5),
        "peer_sub_keys": nrm(ks[18], (DEPTH, PEER_HEADS, 2, N_KEYS, PEER_DK_HALF), PEER_DK_HALF ** -0.5),
        "peer_u": nrm(ks[19], (DEPTH, N_EXPERTS, D_MODEL), D_MODEL ** -0.5),
        "peer_v": nrm(ks[20], (DEPTH, N_EXPERTS, D_MODEL), 0.5),
        "final_norm_g": gain(ks[21], (D_MODEL,)),
    }


def reference(x, meta_tokens, norm_mix_g, w_in, b_in, sinks, conv_w, conv_b, w_r, b_r, w_i, b_i,
              lru_lambda, gn_attn_g, gn_lru_g, w_out, norm_ffn_g, peer_wq, peer_sub_keys,
              peer_u, peer_v, final_norm_g):
    B = x.shape[0]
    meta = jnp.broadcast_to(meta_tokens[None].astype(x.dtype), (B, N_META, D_MODEL))
    h = jnp.concatenate([meta, x], axis=1)
    T = h.shape[1]
    split_at = [Q_DIM, Q_DIM + KV_DIM, Q_DIM + 2 * KV_DIM, Q_DIM + 2 * KV_DIM + LRU_WIDTH]
    for l in range(DEPTH):
        hn = rmsnorm(h, norm_mix_g[l])
        proj = hn @ w_in[l] + b_in[l]
        q, k, v, xb, gate = jnp.split(proj, split_at, axis=-1)
        o_attn = window_attention(q, k, v, sinks[l])
        o_lru = rglru_branch(xb, gate, conv_w[l], conv_b[l], w_r[l], b_r[l], w_i[l], b_i[l],
                             lru_lambda[l])
        mixed = jnp.concatenate([rmsnorm(o_attn, gn_attn_g[l]), rmsnorm(o_lru, gn_lru_g[l])], axis=-1)
        h = h + mixed @ w_out[l]
        hn = rmsnorm(h, norm_ffn_g[l]).reshape(B * T, D_MODEL)
        h = h + peer(hn, peer_wq[l], peer_sub_keys[l], peer_u[l], peer_v[l]).reshape(B, T, D_MODEL)
    y = rmsnorm(h, final_norm_g)
    return y[:, N_META:]
```

```python
import numpy as np
from contextlib import ExitStack
from concourse.bass_utils import run_bass_kernel_spmd
import concourse.bass as bass
import concourse.mybir as mybir

F32 = mybir.dt.float32
BF16 = mybir.dt.bfloat16
I32 = mybir.dt.int32
U32 = mybir.dt.uint32
AF = mybir.ActivationFunctionType
ALU = mybir.AluOpType
AX = mybir.AxisListType

ENGINES = ["sync", "scalar", "vector", "gpsimd", "tensor"]
DMA_SLOTS = 8


class Sched:
    def __init__(self, nc, stack):
        self.nc = nc
        self.stack = stack
        self.streams = {e: [] for e in ENGINES}
        self.esem = {e: stack.enter_context(nc.semaphore("es_" + e)) for e in ENGINES}
        self.cnt = {e: 0 for e in ENGINES}
        self.known = {e: {} for e in ENGINES}
        self.dsem = {}
        self.dcnt = {}
        for q in ["sync", "gpsimd", "scalar"]:
            self.dsem[q] = [stack.enter_context(nc.semaphore("ds_%s_%d" % (q, j)))
                            for j in range(DMA_SLOTS)]
            self.dcnt[q] = 0
        self.sems = {}
        for e in ENGINES:
            self.sems[("e", e)] = self.esem[e]
        for q in self.dsem:
            for j in range(DMA_SLOTS):
                self.sems[("d", q, j)] = self.dsem[q][j]
        self.res = {}
        self.ntens = 0

    def sbuf(self, name, shape, dtype):
        return self.stack.enter_context(self.nc.sbuf_tensor("sb_" + name, list(shape), dtype))

    def psum(self, name, shape, dtype=F32):
        return self.stack.enter_context(self.nc.psum_tensor("pp_" + name, list(shape), dtype))

    @staticmethod
    def key(x):
        if isinstance(x, str):
            return x
        if isinstance(x, tuple) and len(x) == 2 and isinstance(x[1], str):
            return x[1]
        t = getattr(x, "tensor", x)
        return t.name

    def _need(self, eng, reads, writes):
        need = {}

        def add(ev):
            if ev is None:
                return
            k, v = ev
            if eng == "tensor" and k == ("e", "tensor"):
                return
            if need.get(k, 0) < v:
                need[k] = v

        for r in reads:
            st = self.res.get(self.key(r))
            if st is not None:
                add(st["w"])
        for w in writes:
            st = self.res.get(self.key(w))
            if st is not None:
                add(st["w"])
                for ev in st["r"]:
                    add(ev)
        return need

    def _emit_waits(self, eng, need):
        kn = self.known[eng]
        for k, v in need.items():
            if kn.get(k, 0) >= v:
                continue
            kn[k] = v
            sem = self.sems[k]
            self.streams[eng].append(("wait", sem, v))

    def _record(self, ev, reads, writes):
        for r in reads:
            k = self.key(r)
            st = self.res.setdefault(k, {"w": None, "r": []})
            st["r"].append(ev)
            if len(st["r"]) > 64:
                best = {}
                for (kk, vv) in st["r"]:
                    if best.get(kk, 0) < vv:
                        best[kk] = vv
                st["r"] = list(best.items())
        for w in writes:
            k = self.key(w)
            self.res[k] = {"w": ev, "r": []}

    def op(self, eng, fn, reads=(), writes=()):
        need = self._need(eng, reads, writes)
        self._emit_waits(eng, need)
        self.cnt[eng] += 1
        ev = (("e", eng), self.cnt[eng])
        self.streams[eng].append(("op", fn, self.esem[eng], 1))
        self._record(ev, reads, writes)
        return ev

    def dma(self, q, fn, reads=(), writes=()):
        n = self.dcnt[q]
        self.dcnt[q] += 1
        j = n % DMA_SLOTS
        rnd = n // DMA_SLOTS
        need = self._need(q, reads, writes)
        if rnd > 0:
            k = ("d", q, j)
            if need.get(k, 0) < 16 * rnd:
                need[k] = 16 * rnd
        self._emit_waits(q, need)
        ev = (("d", q, j), 16 * (rnd + 1))
        self.streams[q].append(("op", fn, self.dsem[q][j], 16))
        self._record(ev, reads, writes)
        return ev

    def wait_all(self, eng, resources):
        need = self._need(eng, resources, ())
        self._emit_waits(eng, need)

    def emit(self):
        nc = self.nc
        streams = self.streams
        with nc.Block() as block:
            def run(engname):
                def body(e):
                    for item in streams[engname]:
                        if item[0] == "wait":
                            e.wait_ge(item[1], item[2])
                        else:
                            ins = item[1](e)
                            ins.then_inc(item[2], item[3])
                return body
            block.sync(run("sync"))
            block.scalar(run("scalar"))
            block.vector(run("vector"))
            block.gpsimd(run("gpsimd"))
            block.tensor(run("tensor"))

    def mm(self, out, lhsT, rhs, start=True, stop=True, **kw):
        return self.op("tensor", lambda e: e.matmul(out, lhsT, rhs, start=start, stop=stop, **kw),
                       reads=[lhsT, rhs], writes=[out])

    def tr(self, out, in_, ident):
        return self.op("tensor", lambda e: e.transpose(out, in_, ident),
                       reads=[in_, ident], writes=[out])

    def act(self, out, in_, func, bias=None, scale=1.0, accum_out=None, extra_reads=()):
        reads = [in_] + list(extra_reads)
        kw = {}
        if bias is not None:
            kw["bias"] = bias
            if not isinstance(bias, (int, float)):
                reads.append(bias)
        if not isinstance(scale, (int, float)):
            reads.append(scale)
        writes = [out]
        if accum_out is not None:
            kw["accum_out"] = accum_out
            writes.append(accum_out)
        return self.op("scalar", lambda e: e.activation(out, in_, func, scale=scale, **kw),
                       reads=reads, writes=writes)

    def tt(self, eng, out, in0, in1, op):
        return self.op(eng, lambda e: e.tensor_tensor(out, in0, in1, op),
                       reads=[in0, in1], writes=[out])

    def ts(self, eng, out, in0, s1, s2, op0, op1=None, accum_out=None):
        reads = [in0]
        if not isinstance(s1, (int, float)):
            reads.append(s1)
        if s2 is not None and not isinstance(s2, (int, float)):
            reads.append(s2)
        writes = [out]
        kw = {}
        if op1 is not None:
            kw["op1"] = op1
        if accum_out is not None:
            kw["accum_out"] = accum_out
            writes.append(accum_out)
        return self.op(eng, lambda e: e.tensor_scalar(out, in0, s1, s2, op0, **kw),
                       reads=reads, writes=writes)

    def stt(self, out, in0, scalar, in1, op0, op1, accum_out=None, eng="vector"):
        reads = [in0, in1]
        if not isinstance(scalar, (int, float)):
            reads.append(scalar)
        writes = [out]
        kw = {}
        if accum_out is not None:
            kw["accum_out"] = accum_out
            writes.append(accum_out)
        return self.op(eng, lambda e: e.scalar_tensor_tensor(out, in0, scalar, in1, op0, op1, **kw),
                       reads=reads, writes=writes)

    def copy(self, eng, out, in_):
        if eng == "scalar":
            return self.op(eng, lambda e: e.copy(out, in_), reads=[in_], writes=[out])
        return self.op(eng, lambda e: e.tensor_copy(out, in_), reads=[in_], writes=[out])

    def memset(self, eng, out, val):
        return self.op(eng, lambda e: e.memset(out, val), reads=[], writes=[out])

    def load(self, q, out, in_, reads=None, **kw):
        return self.dma(q, lambda e: e.dma_start(out=out, in_=in_, **kw),
                        reads=(reads if reads is not None else []), writes=[out])

    def store(self, q, out, in_, wkey=None, **kw):
        return self.dma(q, lambda e: e.dma_start(out=out, in_=in_, **kw),
                        reads=[in_], writes=([wkey] if wkey else []))

    def barrier(self):
        need = {}
        for e in ENGINES:
            if self.cnt[e] > 0:
                need[("e", e)] = self.cnt[e]
        for q in self.dsem:
            n = self.dcnt[q]
            for j in range(DMA_SLOTS):
                c = (n - j + DMA_SLOTS - 1) // DMA_SLOTS if n > j else 0
                if c > 0:
                    need[("d", q, j)] = 16 * c
        for e in ENGINES:
            self._emit_waits(e, dict(need))

    def recip(self, out, in_, eng="vector"):
        return self.op(eng, lambda e: e.reciprocal(out, in_), reads=[in_], writes=[out])

    def scan(self, out, d0, d1, init, op0, op1):
        reads = [d0, d1]
        if not isinstance(init, (int, float)):
            reads.append(init)
        return self.op("vector", lambda e: e.tensor_tensor_scan(out, d0, d1, init, op0, op1),
                       reads=reads, writes=[out])

    def vmax(self, out, in_):
        return self.op("vector", lambda e: e.max(out, in_), reads=[in_], writes=[out])

    def vmax_index(self, out, in_max, in_values):
        return self.op("vector", lambda e: e.max_index(out, in_max, in_values),
                       reads=[in_max, in_values], writes=[out])

    def vmatch_replace(self, out, in_to_replace, in_values, imm):
        return self.op("vector", lambda e: e.match_replace(out, in_to_replace, in_values, imm),
                       reads=[in_to_replace, in_values], writes=[out])

    def reduce(self, out, in_, axis, op, eng="vector"):
        return self.op(eng, lambda e: e.tensor_reduce(out, in_, axis, op), reads=[in_], writes=[out])

    def gather(self, out, table, idx, extra_reads=()):
        return self.dma("gpsimd", lambda e: e.indirect_dma_start(
            out=out, out_offset=None, in_=table,
            in_offset=bass.IndirectOffsetOnAxis(ap=idx, axis=0)), reads=[idx] + list(extra_reads), writes=[out])


D = 2048
SEQ = 2048
NMETA = 16
T = SEQ + NMETA
EPS = 1e-6
NEXP = 16384
TILES = [(0, NMETA)] + [(NMETA + 128 * i, 128) for i in range(SEQ // 128)]
NT = len(TILES)

PCOL = {}
_o = 0
for _n, _w in [("gmix", 16), ("bq", 8), ("bk", 2), ("bxb", 8), ("bgate", 8), ("cw", 32), ("cb", 8),
               ("br", 8), ("bi", 8), ("lam", 8), ("gr", 8), ("ga", 8)]:
    PCOL[_n] = (_o, _w)
    _o += _w
NPCOL = _o
PROW = {}
_o = 0
for _n, _w in [("bv", 256), ("sink", 16), ("iota", 16), ("thr", 16)]:
    PROW[_n] = (_o, _w)
    _o += _w
NPROW = _o

DEBUG = False


def build_program(n_tiles_c=16):
    nc = bass.Bass("TRN2", target_bir_lowering=False)

    def din(name, shape, dt=F32):
        return nc.dram_tensor(name, list(shape), dt, kind="ExternalInput").ap()

    h0_d = din("h0", [T, D])
    wa_d = din("wa", [D, 1536])
    wl_d = din("wl", [D, 2048])
    wo_d = din("wo", [D, 2048])
    wq_d = din("wq", [D, 2048])
    wri_d = din("wri", [128, 16, 128])
    skT_d = din("skT", [128, 16, 128])
    pcol_d = din("pcol", [128, NPCOL])
    prow_d = din("prow", [128, NPROW])
    gffn_d = din("gffn", [128, D])
    gfin_d = din("gfin", [128, D])
    ident_d = din("ident", [128, 128])
    masks_d = din("masks", [128, 2, 128])
    utab_d = din("utab", [NEXP, D])
    vtab_d = din("vtab", [NEXP, D])
    y_d = nc.dram_tensor("y", [SEQ, D], F32, kind="ExternalOutput").ap()
    h1_d = nc.dram_tensor("h1scr", [SEQ, D], F32, kind="Internal").ap()
    uvtb_d = nc.dram_tensor("uvtb", [NEXP, 2, D], BF16, kind="Internal").ap()
    dbg = {}
    if DEBUG:
        dbg["mixa"] = nc.dram_tensor("dbg_mixa", [128, 8, T], BF16, kind="ExternalOutput").ap()
        dbg["mixr"] = nc.dram_tensor("dbg_mixr", [128, 8, T], BF16, kind="ExternalOutput").ap()
        dbg["rstd"] = nc.dram_tensor("dbg_rstd", [128, 2, NT], F32, kind="ExternalOutput").ap()
        dbg["eidx"] = nc.dram_tensor("dbg_eidx", [128, 128], I32, kind="ExternalOutput").ap()
        dbg["wgt"] = nc.dram_tensor("dbg_wgt", [128, 128], F32, kind="ExternalOutput").ap()

    with ExitStack() as gst:
        S = Sched(nc, gst)
        pcol = S.sbuf("pcol", [128, NPCOL], F32)
        prow = S.sbuf("prow", [128, NPROW], F32)
        identf = S.sbuf("identf", [128, 128], F32)
        identb = S.sbuf("identb", [128, 128], BF16)
        masksf = S.sbuf("masksf", [128, 2, 128], F32)
        masks = S.sbuf("masks", [128, 2, 128], BF16)
        ones_b = S.sbuf("ones_b", [128, 128], BF16)
        ones_f = S.sbuf("ones_f", [128, 2], F32)
        eps_t = S.sbuf("eps_t", [128, 1], F32)
        one_t = S.sbuf("one_t", [128, 1], F32)
        rstd_a = S.sbuf("rstd_a", [128, NT], F32)
        rstd_r = S.sbuf("rstd_r", [128, NT], F32)
        mid = ExitStack()
        S.stack = mid
        mixa = S.sbuf("mixa", [128, 8, T], BF16)
        mixr = S.sbuf("mixr", [128, 8, T], BF16)

        def pc(name, a=0, b=None):
            o, w = PCOL[name]
            b = w if b is None else b
            return pcol[:, o + a:o + b]

        def pr(name, a=0, b=None):
            o, w = PROW[name]
            b = w if b is None else b
            return prow[:, o + a:o + b]

        TB_KEYS = {"u": [], "v": []}
        S.load("sync", pcol[:], pcol_d)
        S.load("sync", prow[:], prow_d)
        S.load("sync", identf[:], ident_d)
        S.load("sync", masksf[:], masks_d)
        S.copy("vector", identb[:], identf[:])
        S.copy("vector", masks[:], masksf[:])
        S.memset("vector", ones_b[:], 1.0)
        S.memset("vector", ones_f[:], 1.0)
        S.memset("vector", eps_t[:], EPS)
        S.memset("vector", one_t[:], 1.0)

        def load_weights(ph_name, w_dram, ncols, n=16, extra=()):
            tiles = [S.sbuf("%s_w%d" % (ph_name, k), [128, ncols], BF16) for k in range(n)]
            outer = S.stack
            with ExitStack() as sub:
                S.stack = sub
                stg = [S.sbuf("%s_stg%d" % (ph_name, i), [128, ncols], F32) for i in range(2)]
                for k in range(n):
                    S.load("sync", stg[k % 2][:], w_dram[k * 128:(k + 1) * 128, :])
                    S.copy("scalar" if k % 2 else "vector", tiles[k][:], stg[k % 2][:])
                for (dst, src_d, shp) in extra:
                    tmp = S.sbuf("%s_xstg_%s" % (ph_name, dst.name), shp, F32)
                    S.load("sync", tmp[:], src_d)
                    S.copy("vector", dst[:], tmp[:])
            S.stack = outer
            S.barrier()
            return tiles

        def norm_T(pfx, xt, ntok, junk, ss, rs, rstd, xn, ps_tr, hnT, gname):
            S.act(junk[:ntok, :], xt[:ntok, :], AF.Square, accum_out=ss[:ntok, 0:1])
            S.act(rs[:ntok, :], ss[:ntok, :], AF.Sqrt, scale=1.0 / D, bias=eps_t[:ntok, :])
            S.recip(rstd[:ntok, :], rs[:ntok, :])
            S.ts("vector", xn[:ntok, :], xt[:ntok, :], rstd[:ntok, 0:1], None, ALU.mult)
            for half in range(2):
                for kk in range(8):
                    k = half * 8 + kk
                    S.tr(ps_tr[:, kk, :ntok], xn[:ntok, k * 128:(k + 1) * 128], identb[:ntok, :ntok])
                if gname is None:
                    S.copy("vector", hnT[:, half * 8:(half + 1) * 8, :ntok], ps_tr[:, :, :ntok])
                else:
                    S.tt("vector", hnT[:, half * 8:(half + 1) * 8, :ntok], ps_tr[:, :, :ntok],
                         pc(gname, half * 8, half * 8 + 8).unsqueeze(2).to_broadcast([128, 8, ntok]),
                         ALU.mult)

        with ExitStack() as ph:
            S.stack = ph
            WA = load_weights("A", wa_d, 1536)
            NCC = 16
            RCC = NEXP // NCC
            cast_keys = []
            for nm, src_d, half_ in (("u", utab_d, 0), ("v", vtab_d, 1)):
                for c in range(NCC):
                    key = "%stb%d" % (nm, c)
                    TB_KEYS[nm].append(key)
                    prev = [cast_keys[-2]] if len(cast_keys) >= 2 else []
                    S.dma("gpsimd", (lambda e, c=c, src_d=src_d, half_=half_:
                                     e.dma_start(out=uvtb_d[c * RCC:(c + 1) * RCC, half_, :],
                                                 in_=src_d[c * RCC:(c + 1) * RCC, :])),
                          reads=[WA[15]] + prev, writes=[key])
                    cast_keys.append(key)
            xts = [S.sbuf("A_xt%d" % i, [128, D], F32) for i in range(2)]
            junk = S.sbuf("A_junk", [128, D], BF16)
            xn = S.sbuf("A_xn", [128, D], BF16)
            ss = S.sbuf("A_ss", [128, 1], F32)
            rs = S.sbuf("A_rs", [128, 1], F32)
            rstd = S.sbuf("A_rstd", [128, 1], F32)
            hnT = S.sbuf("A_hnT", [128, 16, 128], BF16)
            qT = S.sbuf("A_qT", [128, 8, 128], BF16)
            kT = S.sbuf("A_kT", [128, 2, T], BF16)
            vall = S.sbuf("A_v", [128, NT, 256], BF16)
            esink = S.sbuf("A_esink", [128, 16], F32)
            pT = [S.sbuf("A_pT%d" % i, [128, 3, 4, 128], BF16) for i in range(2)]
            den = S.sbuf("A_den", [128, 4, 128], F32)
            o32 = S.sbuf("A_o32", [128, 4, 128], F32)
            sq = S.sbuf("A_sq", [128, 8, 128], F32)
            sst = S.sbuf("A_sst", [128, 1], F32)
            ps_tr = S.psum("A_ps_tr", [128, 8, 128], BF16)
            ps_proj = [S.psum("A_ps_proj%d" % i, [128, 4, 128], F32) for i in range(2)]
            ps_s = S.psum("A_ps_s", [128, 3, 4, 128], F32)
            ps_o = S.psum("A_ps_o", [128, 4, 128], F32)
            ps_sum = S.psum("A_ps_sum", [128, 4, 128], F32)

            S.act(esink[:], pr("sink"), AF.Exp)

            for t, (tok0, ntok) in enumerate(TILES):
                xt = xts[t % 2]
                S.load("sync", xt[:ntok, :], h0_d[tok0:tok0 + ntok, :])
                norm_T("A", xt, ntok, junk, ss, rs, rstd, xn, ps_tr, hnT, "gmix")
                for grp in range(2):
                    pp = ps_proj[grp]
                    for j in range(4):
                        f = grp * 4 + j
                        for k in range(16):
                            S.mm(pp[:, j, :ntok], WA[k][:, f * 128:(f + 1) * 128], hnT[:, k, :ntok],
                                 start=(k == 0), stop=(k == 15))
                    S.tt("vector", qT[:, grp * 4:grp * 4 + 4, :ntok], pp[:, :, :ntok],
                         pc("bq", grp * 4, grp * 4 + 4).unsqueeze(2).to_broadcast([128, 4, ntok]), ALU.add)
                pp = ps_proj[0]
                for g in range(2):
                    for k in range(16):
                        S.mm(pp[:, g, :ntok], WA[k][:, 1024 + g * 128:1024 + (g + 1) * 128], hnT[:, k, :ntok],
                             start=(k == 0), stop=(k == 15))
                S.tt("vector", kT[:, :, tok0:tok0 + ntok], pp[:, 0:2, :ntok],
                     pc("bk").unsqueeze(2).to_broadcast([128, 2, ntok]), ALU.add)
                pv = ps_proj[1]
                pvv = pv[:, 0:2, :].rearrange("p a b -> p (a b)")
                for k in range(16):
                    S.mm(pvv[:ntok, :], hnT[:, k, :ntok], WA[k][:, 1280:1536], start=(k == 0), stop=(k == 15))
                S.tt("vector", vall[:ntok, t, :], pvv[:ntok, :], pr("bv")[:ntok, :], ALU.add)
                if t == 0:
                    parts = [(0, NMETA, 1)]
                elif t == 1:
                    parts = [(0, NMETA, None), (1, 128, 1)]
                else:
                    parts = [(0, NMETA, None), (t - 1, 128, 0), (t, 128, 1)]
                quad = 0
                for g in range(2):
                    for par in range(2):
                        prs = slice(par * 64, par * 64 + 64)
                        pTq = pT[quad % 2]
                        qidx = g * 2 + par
                        full = (ntok == 128)

                        def fl(ap3):
                            return ap3.rearrange("p a b -> p (a b)")
                        for pi, (tt_, nk, m) in enumerate(parts):
                            k0 = TILES[tt_][0]
                            if full:
                                S.mm(fl(ps_s[:nk, pi, :, :]), kT[prs, g, k0:k0 + nk], fl(qT[prs, 4 * g:4 * g + 4, :]),
                                     start=True, stop=True)
                            else:
                                for j in range(4):
                                    S.mm(ps_s[:nk, pi, j, :ntok], kT[prs, g, k0:k0 + nk], qT[prs, 4 * g + j, :ntok],
                                         start=True, stop=True)
                        for pi, (tt_, nk, m) in enumerate(parts):
                            S.act(pTq[:nk, pi, :, :ntok], ps_s[:nk, pi, :, :ntok], AF.Exp, scale=0.125)
                            if m is not None:
                                S.tt("vector", pTq[:nk, pi, :, :ntok], pTq[:nk, pi, :, :ntok],
                                     masks[:nk, m, :ntok].unsqueeze(1).to_broadcast([nk, 4, ntok]), ALU.mult)
                        for (pst, lhs_of) in ((ps_o, lambda tt_, nk: vall[:nk, tt_, g * 128:(g + 1) * 128]),
                                              (ps_sum, lambda tt_, nk: ones_b[:nk, :])):
                            for pi, (tt_, nk, m) in enumerate(parts):
                                st_, sp_ = (pi == 0), (pi == len(parts) - 1)
                                if full:
                                    S.mm(fl(pst[:, :, :]), lhs_of(tt_, nk), fl(pTq[:nk, pi, :, :]), start=st_, stop=sp_)
                                else:
                                    for j in range(4):
                                        S.mm(pst[:, j, :ntok], lhs_of(tt_, nk), pTq[:nk, pi, j, :ntok],
                                             start=st_, stop=sp_)
                        S.tt("vector", den[prs, :, :ntok], ps_sum[prs, :, :ntok],
                             esink[prs, qidx * 4:qidx * 4 + 4].unsqueeze(2).to_broadcast([64, 4, ntok]), ALU.add)
                        S.recip(den[prs, :, :ntok], den[prs, :, :ntok])
                        S.tt("vector", o32[prs, :, :ntok], ps_o[prs, :, :ntok], den[prs, :, :ntok], ALU.mult)
                        S.tt("vector", sq[prs, 4 * g:4 * g + 4, :ntok], o32[prs, :, :ntok], o32[prs, :, :ntok],
                             ALU.mult)
                        S.tt("vector", mixa[prs, 4 * g:4 * g + 4, tok0:tok0 + ntok], o32[prs, :, :ntok],
                             pcol[prs, PCOL["ga"][0] + 4 * g:PCOL["ga"][0] + 4 * g + 4]
                             .unsqueeze(2).to_broadcast([64, 4, ntok]), ALU.mult)
                        quad += 1
                pss = ps_proj[0]
                for i in range(8):
                    S.mm(pss[:ntok, 3, 0:2], sq[:, i, :ntok], ones_f[:, 0:2], start=(i == 0), stop=(i == 7))
                S.act(sst[:ntok, :], pss[:ntok, 3, 0:1], AF.Sqrt, scale=1.0 / 1024, bias=eps_t[:ntok, :])
                S.recip(rstd_a[:ntok, t:t + 1], sst[:ntok, :])
        S.barrier()

        with ExitStack() as ph:
            S.stack = ph
            WL = load_weights("B", wl_d, 2048)
            wri = S.sbuf("B_wri", [128, 16, 128], F32)
            S.load("sync", wri[:], wri_d)
            xt = S.sbuf("B_xt", [128, D], F32)
            xn = S.sbuf("B_xn", [128, D], BF16)
            junk = xn
            ss = S.sbuf("B_ss", [128, 1], F32)
            rs = S.sbuf("B_rs", [128, 1], F32)
            rstd = S.sbuf("B_rstd", [128, 1], F32)
            hnT = S.sbuf("B_hnT", [128, 16, 128], BF16)
            hist = [S.sbuf("B_hist%d" % i, [128, 8, 131], F32) for i in range(2)]
            ggs = [S.sbuf("B_gg%d" % i, [128, 8, 128], F32) for i in range(2)]
            hstate = S.sbuf("B_hstate", [128, 8], F32)
            c8ls = S.sbuf("B_c8ls", [128, 8], F32)
            tmpl = S.sbuf("B_tmpl", [128, 8], F32)
            sst = S.sbuf("B_sst", [128, 1], F32)
            xc = S.sbuf("B_xc", [128, 8, 128], F32)
            tmpw = S.sbuf("B_tmpw", [128, 8, 128], F32)
            rr = S.sbuf("B_rr", [128, 8, 128], F32)
            ig = S.sbuf("B_ig", [128, 8, 128], F32)
            mu = S.sbuf("B_mu", [128, 8, 128], F32)
            hh = S.sbuf("B_hh", [128, 8, 128], F32)
            ps_tr = S.psum("B_ps_tr", [128, 8, 128], BF16)
            ps_xb = [S.psum("B_ps_xb%d" % i, [128, 4, 128], F32) for i in range(2)]
            ps_g = [S.psum("B_ps_g%d" % i, [128, 4, 128], F32) for i in range(2)]
            ps_r = S.psum("B_ps_r", [128, 4, 128], F32)
            ps_i = S.psum("B_ps_i", [128, 4, 128], F32)
            ps_ss = S.psum("B_ps_ss", [128, 512], F32)

            for i in range(2):
                S.memset("vector", hist[i][:], 0.0)
            S.memset("vector", hstate[:], 0.0)
            S.act(tmpl[:], pc("lam"), AF.Exp, scale=-1.0)
            S.act(tmpl[:], tmpl[:], AF.Ln, bias=one_t[:, 0:1])
            S.ts("vector", c8ls[:], tmpl[:], -8.0, None, ALU.mult)
            cwv = pc("cw").rearrange("p (c k) -> p c k", k=4)

            def bc(ap2, n):
                return ap2.unsqueeze(2).to_broadcast([128, ap2.shape[1], n])

            def front(t):
                tok0, ntok = TILES[t]
                hc = hist[t % 2]
                S.load("sync", xt[:ntok, :], h0_d[tok0:tok0 + ntok, :])
                norm_T("B", xt, ntok, junk, ss, rs, rstd, xn, ps_tr, hnT, "gmix")
                if t > 0:
                    pt = TILES[t - 1][1]
                    S.copy("vector", hc[:, :, 0:3], hist[(t - 1) % 2][:, :, pt:pt + 3])
                for b in range(2):
                    for j in range(4):
                        c = b * 4 + j
                        for k in range(16):
                            S.mm(ps_xb[b][:, j, :ntok], WL[k][:, c * 128:(c + 1) * 128], hnT[:, k, :ntok],
                                 start=(k == 0), stop=(k == 15))
                    S.tt("vector", hc[:, 4 * b:4 * b + 4, 3:3 + ntok], ps_xb[b][:, :, :ntok],
                         bc(pc("bxb", 4 * b, 4 * b + 4), ntok), ALU.add)
                gg = ggs[t % 2]
                for b in range(2):
                    for j in range(4):
                        c = b * 4 + j
                        for k in range(16):
                            S.mm(ps_g[b][:, j, :ntok], WL[k][:, 1024 + c * 128:1024 + (c + 1) * 128], hnT[:, k, :ntok],
                                 start=(k == 0), stop=(k == 15))
                    S.tt("vector", gg[:, 4 * b:4 * b + 4, :ntok], ps_g[b][:, :, :ntok],
                         bc(pc("bgate", 4 * b, 4 * b + 4), ntok), ALU.add)
                S.act(gg[:, :, :ntok], gg[:, :, :ntok], AF.Gelu_apprx_tanh)

            def back(t):
                tok0, ntok = TILES[t]
                hc = hist[t % 2]
                gg = ggs[t % 2]
                n = ntok
                S.tt("vector", xc[:, :, :n], hc[:, :, 3:3 + n], bc(cwv[:, :, 3], n), ALU.mult)
                S.tt("vector", xc[:, :, :n], xc[:, :, :n], bc(pc("cb"), n), ALU.add)
                for tap in range(3):
                    S.tt("vector", tmpw[:, :, :n], hc[:, :, tap:tap + n], bc(cwv[:, :, tap], n), ALU.mult)
                    S.tt("vector", xc[:, :, :n], xc[:, :, :n], tmpw[:, :, :n], ALU.add)
                for b in range(2):
                    for j in range(4):
                        c = b * 4 + j
                        S.mm(ps_r[:, j, :n], wri[:, c, :], xc[:, c, :n], start=True, stop=True)
                    S.tt("vector", rr[:, 4 * b:4 * b + 4, :n], ps_r[:, :, :n], bc(pc("br", 4 * b, 4 * b + 4), n), ALU.add)
                    for j in range(4):
                        c = b * 4 + j
                        S.mm(ps_i[:, j, :n], wri[:, 8 + c, :], xc[:, c, :n], start=True, stop=True)
                    S.tt("vector", ig[:, 4 * b:4 * b + 4, :n], ps_i[:, :, :n], bc(pc("bi", 4 * b, 4 * b + 4), n), ALU.add)
                S.act(rr[:, :, :n], rr[:, :, :n], AF.Sigmoid)
                S.act(ig[:, :, :n], ig[:, :, :n], AF.Sigmoid)
                S.tt("vector", rr[:, :, :n], rr[:, :, :n], bc(c8ls[:, :], n), ALU.mult)
                S.act(rr[:, :, :n], rr[:, :, :n], AF.Exp)
                S.act(mu[:, :, :n], rr[:, :, :n], AF.Square)
                S.act(mu[:, :, :n], mu[:, :, :n], AF.Sqrt, scale=-1.0, bias=one_t[:, 0:1])
                S.tt("vector", ig[:, :, :n], ig[:, :, :n], xc[:, :, :n], ALU.mult)
                S.tt("vector", ig[:, :, :n], ig[:, :, :n], mu[:, :, :n], ALU.mult)
                for c in range(8):
                    S.scan(hh[:, c, :n], rr[:, c, :n], ig[:, c, :n], hstate[:, c:c + 1], ALU.mult, ALU.add)
                S.copy("vector", hstate[:, :], hh[:, :, n - 1])
                S.tt("vector", hh[:, :, :n], hh[:, :, :n], gg[:, :, :n], ALU.mult)
                S.tt("vector", tmpw[:, :, :n], hh[:, :, :n], hh[:, :, :n], ALU.mult)
                S.tt("vector", mixr[:, :, tok0:tok0 + n], hh[:, :, :n], bc(pc("gr"), n), ALU.mult)
                for i in range(8):
                    S.mm(ps_ss[:n, 0:2], tmpw[:, i, :n], ones_f[:, 0:2], start=(i == 0), stop=(i == 7))
                S.act(sst[:n, :], ps_ss[:n, 0:1], AF.Sqrt, scale=1.0 / 1024, bias=eps_t[:n, :])
                S.recip(rstd_r[:n, t:t + 1], sst[:n, :])

            front(0)
            for t in range(NT):
                if t + 1 < NT:
                    front(t + 1)
                back(t)
        S.barrier()
        if DEBUG:
            S.store("sync", dbg["mixa"], mixa[:])
            S.store("sync", dbg["mixr"], mixr[:])
            S.store("sync", dbg["rstd"][:, 0, :], rstd_a[:])
            S.store("sync", dbg["rstd"][:, 1, :], rstd_r[:])

        with ExitStack() as ph:
            S.stack = ph
            WO = load_weights("O", wo_d, 2048)
            xts = [S.sbuf("O_xt%d" % i, [128, D], F32) for i in range(2)]
            h1s = [S.sbuf("O_h1%d" % i, [128, D], F32) for i in range(2)]
            psA = [S.psum("O_psA%d" % i, [128, 512], F32) for i in range(2)]
            psR = [S.psum("O_psR%d" % i, [128, 512], F32) for i in range(2)]
            for t in range(1, NT):
                tok0, ntok = TILES[t]
                xt = xts[t % 2]
                h1 = h1s[t % 2]
                S.load("sync", xt[:], h0_d[tok0:tok0 + 128, :])
                for n in range(4):
                    cs = slice(n * 512, (n + 1) * 512)
                    pa = psA[n % 2]
                    pr_ = psR[n % 2]
                    for i in range(8):
                        S.mm(pa[:], mixa[:, i, tok0:tok0 + 128], WO[i][:, cs], start=(i == 0), stop=(i == 7))
                    for i in range(8):
                        S.mm(pr_[:], mixr[:, i, tok0:tok0 + 128], WO[8 + i][:, cs], start=(i == 0), stop=(i == 7))
                    S.stt(h1[:, cs], pa[:], rstd_a[:, t:t + 1], xt[:, cs], ALU.mult, ALU.add)
                    S.stt(h1[:, cs], pr_[:], rstd_r[:, t:t + 1], h1[:, cs], ALU.mult, ALU.add)
                S.store("sync", h1_d[tok0 - NMETA:tok0 - NMETA + 128, :], h1[:], wkey="h1scr%d" % t)
        S.barrier()
        mid.close()

        with ExitStack() as ph:
            S.stack = ph
            skT = S.sbuf("C_skT", [128, 16, 128], BF16)
            WQ = load_weights("Q", wq_d, 2048, extra=[(skT, skT_d, [128, 16, 128])])
            gffn = S.sbuf("C_gffn", [128, D], F32)
            gfin = S.sbuf("C_gfin", [128, D], F32)
            S.load("sync", gffn[:], gffn_d)
            S.load("sync", gfin[:], gfin_d)
            h1s = [S.sbuf("C_h1_%d" % i, [128, D], F32) for i in range(2)]
            hn2s = [S.sbuf("C_hn2_%d" % i, [128, D], F32) for i in range(2)]
            xn = S.sbuf("C_xn", [128, D], BF16)
            junkA = xn
            ss = S.sbuf("C_ss", [128, 1], F32)
            rs = S.sbuf("C_rs", [128, 1], F32)
            rstd = S.sbuf("C_rstd", [128, 1], F32)
            ss2 = S.sbuf("C_ss2", [128, 1], F32)
            rs2 = S.sbuf("C_rs2", [128, 1], F32)
            rstd2 = S.sbuf("C_rstd2", [128, 1], F32)
            hnT = S.sbuf("C_hnT", [128, 16, 128], BF16)
            qT = S.sbuf("C_qT", [128, 16, 128], BF16)
            s_sb = S.sbuf("C_s", [128, 16, 128], F32)
            swork = S.sbuf("C_swork", [128, 128], F32)
            topv = S.sbuf("C_topv", [128, 16, 16], F32)
            topi = S.sbuf("C_topi", [128, 16, 16], U32)
            topif = S.sbuf("C_topif", [128, 16, 16], F32)
            cand = S.sbuf("C_cand", [128, 16, 16], F32)
            cwork = S.sbuf("C_cwork", [128, 16, 16], F32)
            bestv = S.sbuf("C_bestv", [128, 8, 16], F32)
            besti = S.sbuf("C_besti", [128, 8, 16], U32)
            bestif = S.sbuf("C_bestif", [128, 8, 16], F32)
            k1f = S.sbuf("C_k1f", [128, 8, 16], F32)
            k2f = S.sbuf("C_k2f", [128, 8, 16], F32)
            ohv = s_sb[:].rearrange("p (h x) (y k) -> p h (x y) k", x=2, k=16)
            i1f = S.sbuf("C_i1f", [128, 8, 16], F32)
            i2f = S.sbuf("C_i2f", [128, 8, 16], F32)
            ef = S.sbuf("C_ef", [128, 128], F32)
            eis = [S.sbuf("C_ei%d" % i, [128, 128], I32) for i in range(2)]
            gex = S.sbuf("C_gex", [128, 8, 16], F32)
            gsum = S.sbuf("C_gsum", [128, 8], F32)
            gws = [S.sbuf("C_gw%d" % i, [128, 8, 16], F32) for i in range(2)]
            NRG = 8
            avs = [S.sbuf("C_av%d" % i, [128, 1], F32) for i in range(NRG)]
            gas = [S.sbuf("C_ga%d" % i, [128, 1], F32) for i in range(NRG)]
            w2s = [S.sbuf("C_w2%d" % i, [128, 1], F32) for i in range(NRG)]
            NGB, NDG = 6, 6
            gbuf = [S.sbuf("C_gb%d" % i, [128, 2 * D], BF16) for i in range(NGB)]
            uvflat = uvtb_d.rearrange("e two d -> e (two d)")
            TB_ALL = TB_KEYS["u"] + TB_KEYS["v"]
            gctr = [0]

            def next_gbuf():
                b = gbuf[gctr[0] % NGB]
                gctr[0] += 1
                return b
            dgs = [S.sbuf("C_dg%d" % i, [128, 128], BF16) for i in range(NDG)]
            ps_tr = S.psum("C_ps_tr", [128, 8, 128], BF16)
            ps_q = S.psum("C_ps_q", [128, 4, 128], F32)
            ps_sc = S.psum("C_ps_sc", [128, 4, 128], F32)
            ps_acc = [S.psum("C_ps_acc%d" % n, [128, 512], F32) for n in range(4)]
            iota = pr("iota")
            last_t = n_tiles_c

            def prologue_steps(t):
                tok0, ntok = TILES[t]
                r0 = tok0 - NMETA
                h1 = h1s[t % 2]
                hn2 = hn2s[t % 2]
                ei = eis[t % 2]
                gw = gws[t % 2]
                steps = []

                def a0():
                    S.load("sync", h1[:], h1_d[r0:r0 + 128, :], reads=["h1scr%d" % t])
                    S.act(junkA[:], h1[:], AF.Square, accum_out=ss[:, 0:1])
                    S.act(rs[:], ss[:], AF.Sqrt, scale=1.0 / D, bias=eps_t[:])
                steps.append(a0)

                def a1():
                    S.recip(rstd[:], rs[:])
                    S.stt(hn2[:], h1[:], rstd[:, 0:1], gffn[:], ALU.mult, ALU.mult)
                    S.copy("scalar", xn[:], hn2[:])
                steps.append(a1)
                for half in range(2):
                    def a2(half=half):
                        for kk in range(8):
                            k = half * 8 + kk
                            S.tr(ps_tr[:, kk, :], xn[:, k * 128:(k + 1) * 128], identb[:])
                    steps.append(a2)

                    def a3(half=half):
                        S.copy("vector", hnT[:, half * 8:(half + 1) * 8, :], ps_tr[:])
                    steps.append(a3)
                for grp in range(4):
                    def b0(grp=grp):
                        for j in range(4):
                            hp = grp * 4 + j
                            for k in range(16):
                                S.mm(ps_q[:, j, :], WQ[k][:, hp * 128:(hp + 1) * 128], hnT[:, k, :],
                                     start=(k == 0), stop=(k == 15))
                    steps.append(b0)

                    def b1(grp=grp):
                        S.copy("scalar", qT[:, grp * 4:grp * 4 + 4, :], ps_q[:])
                    steps.append(b1)
                for grp in range(4):
                    def c0(grp=grp):
                        for j in range(4):
                            hp = grp * 4 + j
                            S.mm(ps_sc[:, j, :], qT[:, hp, :], skT[:, hp, :], start=True, stop=True)
                    steps.append(c0)

                    def c1(grp=grp):
                        S.copy("scalar", s_sb[:, grp * 4:grp * 4 + 4, :], ps_sc[:])
                    steps.append(c1)
                for hp in range(16):
                    def d0(hp=hp):
                        S.vmax(topv[:, hp, 0:8], s_sb[:, hp, :])
                        S.vmax_index(topi[:, hp, 0:8], topv[:, hp, 0:8], s_sb[:, hp, :])
                        S.vmatch_replace(swork[:], topv[:, hp, 0:8], s_sb[:, hp, :], -1e30)
                        S.vmax(topv[:, hp, 8:16], swork[:])
                        S.vmax_index(topi[:, hp, 8:16], topv[:, hp, 8:16], swork[:])
                    steps.append(d0)

                def d1():
                    S.copy("vector", topif[:], topi[:])
                steps.append(d1)
                for h in range(8):
                    def e0(h=h):
                        S.tt("vector", cand[:], topv[:, 2 * h, :].unsqueeze(2).to_broadcast([128, 16, 16]),
                             topv[:, 2 * h + 1, :].unsqueeze(1).to_broadcast([128, 16, 16]), ALU.add)
                        cflat = cand[:].rearrange("p a b -> p (a b)")
                        wflat = cwork[:].rearrange("p a b -> p (a b)")
                        S.vmax(bestv[:, h, 0:8], cflat)
                        S.vmax_index(besti[:, h, 0:8], bestv[:, h, 0:8], cflat)
                        S.vmatch_replace(wflat, bestv[:, h, 0:8], cflat, -1e30)
                        S.vmax(bestv[:, h, 8:16], wflat)
                        S.vmax_index(besti[:, h, 8:16], bestv[:, h, 8:16], wflat)
                    steps.append(e0)

                def f0():
                    S.copy("vector", bestif[:], besti[:])
                    S.tt("vector", ohv, bestif[:].unsqueeze(3).to_broadcast([128, 8, 16, 16]),
                         pr("thr").unsqueeze(1).unsqueeze(1).to_broadcast([128, 8, 16, 16]), ALU.is_ge)
                    S.reduce(k1f[:], ohv, AX.X, ALU.add)
                    S.stt(k2f[:], k1f[:], -16.0, bestif[:], ALU.mult, ALU.add)
                steps.append(f0)
                tview = topif[:].rearrange("p (h two) k -> p h two k", two=2)
                for (kf, half_, dst) in ((k1f, 0, i1f), (k2f, 1, i2f)):
                    def f1(kf=kf, half_=half_, dst=dst):
                        S.tt("vector", ohv, iota.unsqueeze(1).unsqueeze(1).to_broadcast([128, 8, 16, 16]),
                             kf[:].unsqueeze(3).to_broadcast([128, 8, 16, 16]), ALU.is_equal)
                        S.tt("vector", ohv, ohv,
                             tview[:, :, half_, :].unsqueeze(2).to_broadcast([128, 8, 16, 16]), ALU.mult)
                        S.reduce(dst[:], ohv, AX.X, ALU.add)
                    steps.append(f1)

                def f2():
                    S.stt(ef[:], i1f[:].rearrange("p h k -> p (h k)"), 128.0,
                          i2f[:].rearrange("p h k -> p (h k)"), ALU.mult, ALU.add)
                    S.copy("vector", ei[:], ef[:])
                    S.tt("vector", gex[:], bestv[:], bestv[:, :, 0:1].to_broadcast([128, 8, 16]), ALU.subtract)
                    S.act(gex[:], gex[:], AF.Exp)
                    S.reduce(gsum[:], gex[:], AX.X, ALU.add)
                    S.recip(gsum[:], gsum[:])
                    S.tt("vector", gw[:], gex[:], gsum[:].unsqueeze(2).to_broadcast([128, 8, 16]), ALU.mult)
                steps.append(f2)
                return steps

            def slot(t, sl):
                gb = next_gbuf()
                av, ga, w2, dg = avs[sl % NRG], gas[sl % NRG], w2s[sl % NRG], dgs[sl % NDG]
                S.gather(gb[:], uvflat, eis[t % 2][:, sl:sl + 1], extra_reads=TB_ALL)
                S.stt(gb[:, 0:D], gb[:, 0:D], 1.0, hn2s[t % 2][:], ALU.mult, ALU.mult, accum_out=av[:, 0:1])
                S.act(ga[:], av[:], AF.Gelu_apprx_tanh)
                S.act(w2[:], ga[:], AF.Copy, scale=gws[t % 2][:].rearrange("p h k -> p (h k)")[:, sl:sl + 1])
                S.act(dg[:], identf[:], AF.Copy, scale=w2[:, 0:1])
                for n in range(4):
                    S.mm(ps_acc[n][:], dg[:], gb[:, D + n * 512:D + (n + 1) * 512], start=(sl == 0), stop=(sl == 127))

            def F(t):
                tok0, ntok = TILES[t]
                r0 = tok0 - NMETA
                h1 = h1s[t % 2]
                for n in range(4):
                    cs = slice(n * 512, (n + 1) * 512)
                    S.tt("vector", h1[:, cs], ps_acc[n][:], h1[:, cs], ALU.add)
                S.act(junkA[:], h1[:], AF.Square, accum_out=ss2[:, 0:1])
                S.act(rs2[:], ss2[:], AF.Sqrt, scale=1.0 / D, bias=eps_t[:])
                S.recip(rstd2[:], rs2[:])
                S.stt(h1[:], h1[:], rstd2[:, 0:1], gfin[:], ALU.mult, ALU.mult)
                S.store("sync", y_d[r0:r0 + 128, :], h1[:])

            for st_ in prologue_steps(1):
                st_()
            for t in range(1, last_t + 1):
                steps = prologue_steps(t + 1) if t + 1 <= last_t else []
                sched = {}
                for i, st_ in enumerate(steps):
                    sched.setdefault(min(127, (i * 100) // max(1, len(steps))), []).append(st_)
                for sl in range(128):
                    slot(t, sl)
                    for st_ in sched.get(sl, []):
                        st_()
                F(t)
        S.barrier()
        S.stack = gst
        S.emit()
    return nc


def _prep_shared(inp):
    f = np.float32
    w_in = np.asarray(inp["w_in"][0], f)
    b_in = np.asarray(inp["b_in"][0], f)
    q_w, q_b = w_in[:, 0:1024], b_in[0:1024]
    k0_w, k1_w = w_in[:, 1024:1088], w_in[:, 1088:1152]
    v0_w, v1_w = w_in[:, 1152:1216], w_in[:, 1216:1280]
    k0_b, k1_b = b_in[1024:1088], b_in[1088:1152]
    v0_b, v1_b = b_in[1152:1216], b_in[1216:1280]
    wa = np.ascontiguousarray(np.concatenate([q_w, k0_w, k0_w, k1_w, k1_w, v0_w, v0_w, v1_w, v1_w], axis=1))
    wl = np.ascontiguousarray(w_in[:, 1280:3328])
    pcol = np.zeros((128, NPCOL), f)

    def put(name, arr):
        o, w = PCOL[name]
        assert arr.shape == (128, w), (name, arr.shape)
        pcol[:, o:o + w] = arr

    def colmajor(v):
        return np.ascontiguousarray(np.asarray(v, f).reshape(-1, 128).T)

    put("gmix", colmajor(inp["norm_mix_g"][0]))
    put("bq", colmajor(q_b))
    put("bk", np.stack([np.concatenate([k0_b, k0_b]), np.concatenate([k1_b, k1_b])], axis=1))
    put("bxb", colmajor(b_in[1280:2304]))
    put("bgate", colmajor(b_in[2304:3328]))
    cw = np.asarray(inp["conv_w"][0], f)
    cwl = cw.T.reshape(8, 128, 4).transpose(1, 0, 2).reshape(128, 32)
    put("cw", np.ascontiguousarray(cwl))
    put("cb", colmajor(inp["conv_b"][0]))
    put("br", colmajor(np.asarray(inp["b_r"][0], f).reshape(-1)))
    put("bi", colmajor(np.asarray(inp["b_i"][0], f).reshape(-1)))
    put("lam", colmajor(inp["lru_lambda"][0]))
    put("gr", colmajor(inp["gn_lru_g"][0]))
    put("ga", colmajor(inp["gn_attn_g"][0]))
    prow = np.zeros((128, NPROW), f)
    o, w = PROW["bv"]
    prow[:, o:o + w] = np.concatenate([v0_b, v0_b, v1_b, v1_b])[None, :]
    sinks = np.asarray(inp["sinks"][0], f)
    sq = np.zeros(16, f)
    for g in range(2):
        for par in range(2):
            for j in range(4):
                sq[(g * 2 + par) * 4 + j] = sinks[8 * g + 2 * j + par]
    o, w = PROW["sink"]
    prow[:, o:o + w] = sq[None, :]
    o, w = PROW["iota"]
    prow[:, o:o + w] = np.arange(16, dtype=f)[None, :]
    o, w = PROW["thr"]
    prow[:, o:o + w] = np.arange(16, 272, 16, dtype=f)[None, :]
    w_r = np.asarray(inp["w_r"][0], f)
    w_i = np.asarray(inp["w_i"][0], f)
    wri = np.zeros((128, 16, 128), f)
    for c in range(8):
        for half in range(2):
            sl = slice(half * 64, half * 64 + 64)
            wri[sl, c, sl] = w_r[2 * c + half]
            wri[sl, 8 + c, sl] = w_i[2 * c + half]
    sk = np.asarray(inp["peer_sub_keys"][0], f)
    skT = np.ascontiguousarray(sk.reshape(16, 128, 128).transpose(2, 0, 1))
    masks = np.zeros((128, 2, 128), f)
    jj = np.arange(128)[:, None]
    ii = np.arange(128)[None, :]
    masks[:, 0, :] = (jj > ii)
    masks[:, 1, :] = (jj <= ii)
    shared = {
        "wa": wa, "wl": wl,
        "wo": np.ascontiguousarray(np.asarray(inp["w_out"][0], f)),
        "wq": np.ascontiguousarray(np.asarray(inp["peer_wq"][0], f)),
        "wri": wri, "skT": skT, "pcol": pcol, "prow": prow,
        "gffn": np.ascontiguousarray(np.broadcast_to(np.asarray(inp["norm_ffn_g"][0], f)[None, :], (128, D))),
        "gfin": np.ascontiguousarray(np.broadcast_to(np.asarray(inp["final_norm_g"], f)[None, :], (128, D))),
        "ident": np.eye(128, dtype=f), "masks": masks,
        "utab": np.ascontiguousarray(np.asarray(inp["peer_u"][0], f)),
        "vtab": np.ascontiguousarray(np.asarray(inp["peer_v"][0], f)),
    }
    return shared


_NC_CACHE = {}


def kernel(**inputs):
    x = np.asarray(inputs["x"], np.float32)
    meta = np.asarray(inputs["meta_tokens"], np.float32)
    B = x.shape[0]
    shared = _prep_shared(inputs)
    if "nc" not in _NC_CACHE:
        _NC_CACHE["nc"] = build_program()
    nc = _NC_CACHE["nc"]
    in_maps = []
    for b in range(B):
        m = dict(shared)
        m["h0"] = np.ascontiguousarray(np.concatenate([meta, x[b]], axis=0))
        in_maps.append(m)
    res = run_bass_kernel_spmd(nc, in_maps, core_ids=list(range(B)))
    out = np.stack([np.asarray(r["y"], np.float32) for r in res.results], axis=0)
    return out
```

```python
import numpy as np
from contextlib import ExitStack
from concourse.bass_utils import run_bass_kernel_spmd
import concourse.bass as bass
import concourse.mybir as mybir

F32 = mybir.dt.float32
BF16 = mybir.dt.bfloat16
I32 = mybir.dt.int32
U32 = mybir.dt.uint32
AF = mybir.ActivationFunctionType
ALU = mybir.AluOpType
AX = mybir.AxisListType

ENGINES = ["sync", "scalar", "vector", "gpsimd", "tensor"]
DMA_SLOTS = 8


class Sched:
    def __init__(self, nc, stack):
        self.nc = nc
        self.stack = stack
        self.streams = {e: [] for e in ENGINES}
        self.esem = {e: stack.enter_context(nc.semaphore("es_" + e)) for e in ENGINES}
        self.cnt = {e: 0 for e in ENGINES}
        self.known = {e: {} for e in ENGINES}
        self.dsem = {}
        self.dcnt = {}
        for q in ["sync", "gpsimd", "scalar"]:
            self.dsem[q] = [stack.enter_context(nc.semaphore("ds_%s_%d" % (q, j)))
                            for j in range(DMA_SLOTS)]
            self.dcnt[q] = 0
        self.sems = {}
        for e in ENGINES:
            self.sems[("e", e)] = self.esem[e]
        for q in self.dsem:
            for j in range(DMA_SLOTS):
                self.sems[("d", q, j)] = self.dsem[q][j]
        self.res = {}
        self.ntens = 0

    def sbuf(self, name, shape, dtype):
        return self.stack.enter_context(self.nc.sbuf_tensor("sb_" + name, list(shape), dtype))

    def psum(self, name, shape, dtype=F32):
        return self.stack.enter_context(self.nc.psum_tensor("pp_" + name, list(shape), dtype))

    @staticmethod
    def key(x):
        if isinstance(x, str):
            return x
        if isinstance(x, tuple) and len(x) == 2 and isinstance(x[1], str):
            return x[1]
        t = getattr(x, "tensor", x)
        return t.name

    def _need(self, eng, reads, writes):
        need = {}

        def add(ev):
            if ev is None:
                return
            k, v = ev
            if eng == "tensor" and k == ("e", "tensor"):
                return
            if need.get(k, 0) < v:
                need[k] = v

        for r in reads:
            st = self.res.get(self.key(r))
            if st is not None:
                add(st["w"])
        for w in writes:
            st = self.res.get(self.key(w))
            if st is not None:
                add(st["w"])
                for ev in st["r"]:
                    add(ev)
        return need

    def _emit_waits(self, eng, need):
        kn = self.known[eng]
        for k, v in need.items():
            if kn.get(k, 0) >= v:
                continue
            kn[k] = v
            sem = self.sems[k]
            self.streams[eng].append(("wait", sem, v))

    def _record(self, ev, reads, writes):
        for r in reads:
            k = self.key(r)
            st = self.res.setdefault(k, {"w": None, "r": []})
            st["r"].append(ev)
            if len(st["r"]) > 64:
                best = {}
                for (kk, vv) in st["r"]:
                    if best.get(kk, 0) < vv:
                        best[kk] = vv
                st["r"] = list(best.items())
        for w in writes:
            k = self.key(w)
            self.res[k] = {"w": ev, "r": []}

    def op(self, eng, fn, reads=(), writes=()):
        need = self._need(eng, reads, writes)
        self._emit_waits(eng, need)
        self.cnt[eng] += 1
        ev = (("e", eng), self.cnt[eng])
        self.streams[eng].append(("op", fn, self.esem[eng], 1))
        self._record(ev, reads, writes)
        return ev

    def dma(self, q, fn, reads=(), writes=()):
        n = self.dcnt[q]
        self.dcnt[q] += 1
        j = n % DMA_SLOTS
        rnd = n // DMA_SLOTS
        need = self._need(q, reads, writes)
        if rnd > 0:
            k = ("d", q, j)
            if need.get(k, 0) < 16 * rnd:
                need[k] = 16 * rnd
        self._emit_waits(q, need)
        ev = (("d", q, j), 16 * (rnd + 1))
        self.streams[q].append(("op", fn, self.dsem[q][j], 16))
        self._record(ev, reads, writes)
        return ev

    def wait_all(self, eng, resources):
        need = self._need(eng, resources, ())
        self._emit_waits(eng, need)

    def emit(self):
        nc = self.nc
        streams = self.streams
        with nc.Block() as block:
            def run(engname):
                def body(e):
                    for item in streams[engname]:
                        if item[0] == "wait":
                            e.wait_ge(item[1], item[2])
                        else:
                            ins = item[1](e)
                            ins.then_inc(item[2], item[3])
                return body
            block.sync(run("sync"))
            block.scalar(run("scalar"))
            block.vector(run("vector"))
            block.gpsimd(run("gpsimd"))
            block.tensor(run("tensor"))

    def mm(self, out, lhsT, rhs, start=True, stop=True, **kw):
        return self.op("tensor", lambda e: e.matmul(out, lhsT, rhs, start=start, stop=stop, **kw),
                       reads=[lhsT, rhs], writes=[out])

    def tr(self, out, in_, ident):
        return self.op("tensor", lambda e: e.transpose(out, in_, ident),
                       reads=[in_, ident], writes=[out])

    def act(self, out, in_, func, bias=None, scale=1.0, accum_out=None, extra_reads=()):
        reads = [in_] + list(extra_reads)
        kw = {}
        if bias is not None:
            kw["bias"] = bias
            if not isinstance(bias, (int, float)):
                reads.append(bias)
        if not isinstance(scale, (int, float)):
            reads.append(scale)
        writes = [out]
        if accum_out is not None:
            kw["accum_out"] = accum_out
            writes.append(accum_out)
        return self.op("scalar", lambda e: e.activation(out, in_, func, scale=scale, **kw),
                       reads=reads, writes=writes)

    def tt(self, eng, out, in0, in1, op):
        return self.op(eng, lambda e: e.tensor_tensor(out, in0, in1, op),
                       reads=[in0, in1], writes=[out])

    def ts(self, eng, out, in0, s1, s2, op0, op1=None, accum_out=None):
        reads = [in0]
        if not isinstance(s1, (int, float)):
            reads.append(s1)
        if s2 is not None and not isinstance(s2, (int, float)):
            reads.append(s2)
        writes = [out]
        kw = {}
        if op1 is not None:
            kw["op1"] = op1
        if accum_out is not None:
            kw["accum_out"] = accum_out
            writes.append(accum_out)
        return self.op(eng, lambda e: e.tensor_scalar(out, in0, s1, s2, op0, **kw),
                       reads=reads, writes=writes)

    def stt(self, out, in0, scalar, in1, op0, op1, accum_out=None, eng="vector"):
        reads = [in0, in1]
        if not isinstance(scalar, (int, float)):
            reads.append(scalar)
        writes = [out]
        kw = {}
        if accum_out is not None:
            kw["accum_out"] = accum_out
            writes.append(accum_out)
        return self.op(eng, lambda e: e.scalar_tensor_tensor(out, in0, scalar, in1, op0, op1, **kw),
                       reads=reads, writes=writes)

    def copy(self, eng, out, in_):
        if eng == "scalar":
            return self.op(eng, lambda e: e.copy(out, in_), reads=[in_], writes=[out])
        return self.op(eng, lambda e: e.tensor_copy(out, in_), reads=[in_], writes=[out])

    def memset(self, eng, out, val):
        return self.op(eng, lambda e: e.memset(out, val), reads=[], writes=[out])

    def load(self, q, out, in_, reads=None, **kw):
        return self.dma(q, lambda e: e.dma_start(out=out, in_=in_, **kw),
                        reads=(reads if reads is not None else []), writes=[out])

    def store(self, q, out, in_, wkey=None, **kw):
        return self.dma(q, lambda e: e.dma_start(out=out, in_=in_, **kw),
                        reads=[in_], writes=([wkey] if wkey else []))

    def barrier(self):
        need = {}
        for e in ENGINES:
            if self.cnt[e] > 0:
                need[("e", e)] = self.cnt[e]
        for q in self.dsem:
            n = self.dcnt[q]
            for j in range(DMA_SLOTS):
                c = (n - j + DMA_SLOTS - 1) // DMA_SLOTS if n > j else 0
                if c > 0:
                    need[("d", q, j)] = 16 * c
        for e in ENGINES:
            self._emit_waits(e, dict(need))

    def recip(self, out, in_, eng="vector"):
        return self.op(eng, lambda e: e.reciprocal(out, in_), reads=[in_], writes=[out])

    def scan(self, out, d0, d1, init, op0, op1):
        reads = [d0, d1]
        if not isinstance(init, (int, float)):
            reads.append(init)
        return self.op("vector", lambda e: e.tensor_tensor_scan(out, d0, d1, init, op0, op1),
                       reads=reads, writes=[out])

    def vmax(self, out, in_):
        return self.op("vector", lambda e: e.max(out, in_), reads=[in_], writes=[out])

    def vmax_index(self, out, in_max, in_values):
        return self.op("vector", lambda e: e.max_index(out, in_max, in_values),
                       reads=[in_max, in_values], writes=[out])

    def vmatch_replace(self, out, in_to_replace, in_values, imm):
        return self.op("vector", lambda e: e.match_replace(out, in_to_replace, in_values, imm),
                       reads=[in_to_replace, in_values], writes=[out])

    def reduce(self, out, in_, axis, op, eng="vector"):
        return self.op(eng, lambda e: e.tensor_reduce(out, in_, axis, op), reads=[in_], writes=[out])

    def gather(self, out, table, idx, extra_reads=()):
        return self.dma("gpsimd", lambda e: e.indirect_dma_start(
            out=out, out_offset=None, in_=table,
            in_offset=bass.IndirectOffsetOnAxis(ap=idx, axis=0)), reads=[idx] + list(extra_reads), writes=[out])


D = 2048
SEQ = 2048
NMETA = 16
T = SEQ + NMETA
EPS = 1e-6
NEXP = 16384
TILES = [(0, NMETA)] + [(NMETA + 128 * i, 128) for i in range(SEQ // 128)]
NT = len(TILES)

PCOL = {}
_o = 0
for _n, _w in [("gmix", 16), ("bq", 8), ("bk", 2), ("bxb", 8), ("bgate", 8), ("cw", 32), ("cb", 8),
               ("br", 8), ("bi", 8), ("lam", 8), ("gr", 8), ("ga", 8)]:
    PCOL[_n] = (_o, _w)
    _o += _w
NPCOL = _o
PROW = {}
_o = 0
for _n, _w in [("bv", 256), ("sink", 16), ("iota", 16), ("thr", 16)]:
    PROW[_n] = (_o, _w)
    _o += _w
NPROW = _o

DEBUG = False


def build_program(n_tiles_c=16):
    nc = bass.Bass("TRN2", target_bir_lowering=False)

    def din(name, shape, dt=F32):
        return nc.dram_tensor(name, list(shape), dt, kind="ExternalInput").ap()

    h0_d = din("h0", [T, D])
    wa_d = din("wa", [D, 1536])
    wl_d = din("wl", [D, 2048])
    wo_d = din("wo", [D, 2048])
    wq_d = din("wq", [D, 2048])
    wri_d = din("wri", [128, 16, 128])
    skT_d = din("skT", [128, 16, 128])
    pcol_d = din("pcol", [128, NPCOL])
    prow_d = din("prow", [128, NPROW])
    gffn_d = din("gffn", [128, D])
    gfin_d = din("gfin", [128, D])
    ident_d = din("ident", [128, 128])
    masks_d = din("masks", [128, 2, 128])
    utab_d = din("utab", [NEXP, D])
    vtab_d = din("vtab", [NEXP, D])
    y_d = nc.dram_tensor("y", [SEQ, D], F32, kind="ExternalOutput").ap()
    h1_d = nc.dram_tensor("h1scr", [SEQ, D], F32, kind="Internal").ap()
    uvtb_d = nc.dram_tensor("uvtb", [NEXP, 2, D], BF16, kind="Internal").ap()
    dbg = {}
    if DEBUG:
        dbg["mixa"] = nc.dram_tensor("dbg_mixa", [128, 8, T], BF16, kind="ExternalOutput").ap()
        dbg["mixr"] = nc.dram_tensor("dbg_mixr", [128, 8, T], BF16, kind="ExternalOutput").ap()
        dbg["rstd"] = nc.dram_tensor("dbg_rstd", [128, 2, NT], F32, kind="ExternalOutput").ap()
        dbg["eidx"] = nc.dram_tensor("dbg_eidx", [128, 128], I32, kind="ExternalOutput").ap()
        dbg["wgt"] = nc.dram_tensor("dbg_wgt", [128, 128], F32, kind="ExternalOutput").ap()

    with ExitStack() as gst:
        S = Sched(nc, gst)
        pcol = S.sbuf("pcol", [128, NPCOL], F32)
        prow = S.sbuf("prow", [128, NPROW], F32)
        identf = S.sbuf("identf", [128, 128], F32)
        identb = S.sbuf("identb", [128, 128], BF16)
        masksf = S.sbuf("masksf", [128, 2, 128], F32)
        masks = S.sbuf("masks", [128, 2, 128], BF16)
        ones_b = S.sbuf("ones_b", [128, 128], BF16)
        ones_f = S.sbuf("ones_f", [128, 2], F32)
        eps_t = S.sbuf("eps_t", [128, 1], F32)
        one_t = S.sbuf("one_t", [128, 1], F32)
        rstd_a = S.sbuf("rstd_a", [128, NT], F32)
        rstd_r = S.sbuf("rstd_r", [128, NT], F32)
        mid = ExitStack()
        S.stack = mid
        mixa = S.sbuf("mixa", [128, 8, T], BF16)
        mixr = S.sbuf("mixr", [128, 8, T], BF16)

        def pc(name, a=0, b=None):
            o, w = PCOL[name]
            b = w if b is None else b
            return pcol[:, o + a:o + b]

        def pr(name, a=0, b=None):
            o, w = PROW[name]
            b = w if b is None else b
            return prow[:, o + a:o + b]

        TB_KEYS = {"u": [], "v": []}
        S.load("sync", pcol[:], pcol_d)
        S.load("sync", prow[:], prow_d)
        S.load("sync", identf[:], ident_d)
        S.load("sync", masksf[:], masks_d)
        S.copy("vector", identb[:], identf[:])
        S.copy("vector", masks[:], masksf[:])
        S.memset("vector", ones_b[:], 1.0)
        S.memset("vector", ones_f[:], 1.0)
        S.memset("vector", eps_t[:], EPS)
        S.memset("vector", one_t[:], 1.0)

        def load_weights(ph_name, w_dram, ncols, n=16, extra=()):
            tiles = [S.sbuf("%s_w%d" % (ph_name, k), [128, ncols], BF16) for k in range(n)]
            outer = S.stack
            with ExitStack() as sub:
                S.stack = sub
                NSTG = 4
                stg = [S.sbuf("%s_stg%d" % (ph_name, i), [128, ncols], F32) for i in range(NSTG)]
                for k in range(n):
                    S.load("sync", stg[k % NSTG][:], w_dram[k * 128:(k + 1) * 128, :])
                    S.copy("scalar" if k % 2 else "vector", tiles[k][:], stg[k % NSTG][:])
                for (dst, src_d, shp) in extra:
                    tmp = S.sbuf("%s_xstg_%s" % (ph_name, dst.name), shp, F32)
                    S.load("sync", tmp[:], src_d)
                    S.copy("vector", dst[:], tmp[:])
            S.stack = outer
            S.barrier()
            return tiles

        def norm_T(pfx, xt, ntok, junk, ss, rs, rstd, xn, ps_tr, hnT, gname):
            S.act(junk[:ntok, :], xt[:ntok, :], AF.Square, accum_out=ss[:ntok, 0:1])
            S.act(rs[:ntok, :], ss[:ntok, :], AF.Sqrt, scale=1.0 / D, bias=eps_t[:ntok, :])
            S.recip(rstd[:ntok, :], rs[:ntok, :])
            S.ts("vector", xn[:ntok, :], xt[:ntok, :], rstd[:ntok, 0:1], None, ALU.mult)
            for half in range(2):
                for kk in range(8):
                    k = half * 8 + kk
                    S.tr(ps_tr[:, kk, :ntok], xn[:ntok, k * 128:(k + 1) * 128], identb[:ntok, :ntok])
                if gname is None:
                    S.copy("vector", hnT[:, half * 8:(half + 1) * 8, :ntok], ps_tr[:, :, :ntok])
                else:
                    S.tt("vector", hnT[:, half * 8:(half + 1) * 8, :ntok], ps_tr[:, :, :ntok],
                         pc(gname, half * 8, half * 8 + 8).unsqueeze(2).to_broadcast([128, 8, ntok]),
                         ALU.mult)

        with ExitStack() as ph:
            S.stack = ph
            WA = load_weights("A", wa_d, 1536)
            NCC = 16
            RCC = NEXP // NCC
            cast_keys = []
            for nm, src_d, half_ in (("u", utab_d, 0), ("v", vtab_d, 1)):
                for c in range(NCC):
                    key = "%stb%d" % (nm, c)
                    TB_KEYS[nm].append(key)
                    prev = [cast_keys[-2]] if len(cast_keys) >= 2 else []
                    S.dma("gpsimd", (lambda e, c=c, src_d=src_d, half_=half_:
                                     e.dma_start(out=uvtb_d[c * RCC:(c + 1) * RCC, half_, :],
                                                 in_=src_d[c * RCC:(c + 1) * RCC, :])),
                          reads=[WA[15]] + prev, writes=[key])
                    cast_keys.append(key)
            xts = [S.sbuf("A_xt%d" % i, [128, D], F32) for i in range(2)]
            junk = S.sbuf("A_junk", [128, D], BF16)
            xn = S.sbuf("A_xn", [128, D], BF16)
            ss = S.sbuf("A_ss", [128, 1], F32)
            rs = S.sbuf("A_rs", [128, 1], F32)
            rstd = S.sbuf("A_rstd", [128, 1], F32)
            hnT = S.sbuf("A_hnT", [128, 16, 128], BF16)
            qT = S.sbuf("A_qT", [128, 8, 128], BF16)
            kT = S.sbuf("A_kT", [128, 2, T], BF16)
            vall = S.sbuf("A_v", [128, NT, 256], BF16)
            esink = S.sbuf("A_esink", [128, 16], F32)
            pT = [S.sbuf("A_pT%d" % i, [128, 3, 4, 128], BF16) for i in range(2)]
            den = S.sbuf("A_den", [128, 4, 128], F32)
            o32 = S.sbuf("A_o32", [128, 4, 128], F32)
            sq = S.sbuf("A_sq", [128, 8, 128], F32)
            sst = S.sbuf("A_sst", [128, 1], F32)
            ps_tr = S.psum("A_ps_tr", [128, 8, 128], BF16)
            ps_proj = [S.psum("A_ps_proj%d" % i, [128, 4, 128], F32) for i in range(2)]
            ps_s = S.psum("A_ps_s", [128, 3, 4, 128], F32)
            ps_o = S.psum("A_ps_o", [128, 4, 128], F32)
            ps_sum = S.psum("A_ps_sum", [128, 4, 128], F32)

            S.act(esink[:], pr("sink"), AF.Exp)

            for t, (tok0, ntok) in enumerate(TILES):
                xt = xts[t % 2]
                S.load("sync", xt[:ntok, :], h0_d[tok0:tok0 + ntok, :])
                norm_T("A", xt, ntok, junk, ss, rs, rstd, xn, ps_tr, hnT, "gmix")
                for grp in range(2):
                    pp = ps_proj[grp]
                    for j in range(4):
                        f = grp * 4 + j
                        for k in range(16):
                            S.mm(pp[:, j, :ntok], WA[k][:, f * 128:(f + 1) * 128], hnT[:, k, :ntok],
                                 start=(k == 0), stop=(k == 15))
                    S.tt("vector", qT[:, grp * 4:grp * 4 + 4, :ntok], pp[:, :, :ntok],
                         pc("bq", grp * 4, grp * 4 + 4).unsqueeze(2).to_broadcast([128, 4, ntok]), ALU.add)
                pp = ps_proj[0]
                for g in range(2):
                    for k in range(16):
                        S.mm(pp[:, g, :ntok], WA[k][:, 1024 + g * 128:1024 + (g + 1) * 128], hnT[:, k, :ntok],
                             start=(k == 0), stop=(k == 15))
                S.tt("vector", kT[:, :, tok0:tok0 + ntok], pp[:, 0:2, :ntok],
                     pc("bk").unsqueeze(2).to_broadcast([128, 2, ntok]), ALU.add)
                pv = ps_proj[1]
                pvv = pv[:, 0:2, :].rearrange("p a b -> p (a b)")
                for k in range(16):
                    S.mm(pvv[:ntok, :], hnT[:, k, :ntok], WA[k][:, 1280:1536], start=(k == 0), stop=(k == 15))
                S.tt("vector", vall[:ntok, t, :], pvv[:ntok, :], pr("bv")[:ntok, :], ALU.add)
                if t == 0:
                    parts = [(0, NMETA, 1)]
                elif t == 1:
                    parts = [(0, NMETA, None), (1, 128, 1)]
                else:
                    parts = [(0, NMETA, None), (t - 1, 128, 0), (t, 128, 1)]
                quad = 0
                for g in range(2):
                    for par in range(2):
                        prs = slice(par * 64, par * 64 + 64)
                        pTq = pT[quad % 2]
                        qidx = g * 2 + par
                        full = (ntok == 128)

                        def fl(ap3):
                            return ap3.rearrange("p a b -> p (a b)")
                        for pi, (tt_, nk, m) in enumerate(parts):
                            k0 = TILES[tt_][0]
                            if full:
                                S.mm(fl(ps_s[:nk, pi, :, :]), kT[prs, g, k0:k0 + nk], fl(qT[prs, 4 * g:4 * g + 4, :]),
                                     start=True, stop=True)
                            else:
                                for j in range(4):
                                    S.mm(ps_s[:nk, pi, j, :ntok], kT[prs, g, k0:k0 + nk], qT[prs, 4 * g + j, :ntok],
                                         start=True, stop=True)
                        for pi, (tt_, nk, m) in enumerate(parts):
                            S.act(pTq[:nk, pi, :, :ntok], ps_s[:nk, pi, :, :ntok], AF.Exp, scale=0.125)
                            if m is not None:
                                S.tt("vector", pTq[:nk, pi, :, :ntok], pTq[:nk, pi, :, :ntok],
                                     masks[:nk, m, :ntok].unsqueeze(1).to_broadcast([nk, 4, ntok]), ALU.mult)
                        for (pst, lhs_of) in ((ps_o, lambda tt_, nk: vall[:nk, tt_, g * 128:(g + 1) * 128]),
                                              (ps_sum, lambda tt_, nk: ones_b[:nk, :])):
                            for pi, (tt_, nk, m) in enumerate(parts):
                                st_, sp_ = (pi == 0), (pi == len(parts) - 1)
                                if full:
                                    S.mm(fl(pst[:, :, :]), lhs_of(tt_, nk), fl(pTq[:nk, pi, :, :]), start=st_, stop=sp_)
                                else:
                                    for j in range(4):
                                        S.mm(pst[:, j, :ntok], lhs_of(tt_, nk), pTq[:nk, pi, j, :ntok],
                                             start=st_, stop=sp_)
                        S.tt("vector", den[prs, :, :ntok], ps_sum[prs, :, :ntok],
                             esink[prs, qidx * 4:qidx * 4 + 4].unsqueeze(2).to_broadcast([64, 4, ntok]), ALU.add)
                        S.recip(den[prs, :, :ntok], den[prs, :, :ntok])
                        S.tt("vector", o32[prs, :, :ntok], ps_o[prs, :, :ntok], den[prs, :, :ntok], ALU.mult)
                        S.tt("vector", sq[prs, 4 * g:4 * g + 4, :ntok], o32[prs, :, :ntok], o32[prs, :, :ntok],
                             ALU.mult)
                        S.tt("vector", mixa[prs, 4 * g:4 * g + 4, tok0:tok0 + ntok], o32[prs, :, :ntok],
                             pcol[prs, PCOL["ga"][0] + 4 * g:PCOL["ga"][0] + 4 * g + 4]
                             .unsqueeze(2).to_broadcast([64, 4, ntok]), ALU.mult)
                        quad += 1
                pss = ps_proj[0]
                for i in range(8):
                    S.mm(pss[:ntok, 3, 0:2], sq[:, i, :ntok], ones_f[:, 0:2], start=(i == 0), stop=(i == 7))
                S.act(sst[:ntok, :], pss[:ntok, 3, 0:1], AF.Sqrt, scale=1.0 / 1024, bias=eps_t[:ntok, :])
                S.recip(rstd_a[:ntok, t:t + 1], sst[:ntok, :])
        S.barrier()

        with ExitStack() as ph:
            S.stack = ph
            WL = load_weights("B", wl_d, 2048)
            wri = S.sbuf("B_wri", [128, 16, 128], F32)
            S.load("sync", wri[:], wri_d)
            xt = S.sbuf("B_xt", [128, D], F32)
            xn = S.sbuf("B_xn", [128, D], BF16)
            junk = xn
            ss = S.sbuf("B_ss", [128, 1], F32)
            rs = S.sbuf("B_rs", [128, 1], F32)
            rstd = S.sbuf("B_rstd", [128, 1], F32)
            hnT = S.sbuf("B_hnT", [128, 16, 128], BF16)
            hist = [S.sbuf("B_hist%d" % i, [128, 8, 131], F32) for i in range(2)]
            ggs = [S.sbuf("B_gg%d" % i, [128, 8, 128], F32) for i in range(2)]
            hstate = S.sbuf("B_hstate", [128, 8], F32)
            c8ls = S.sbuf("B_c8ls", [128, 8], F32)
            tmpl = S.sbuf("B_tmpl", [128, 8], F32)
            sst = S.sbuf("B_sst", [128, 1], F32)
            xc = S.sbuf("B_xc", [128, 8, 128], F32)
            tmpw = S.sbuf("B_tmpw", [128, 8, 128], F32)
            rr = S.sbuf("B_rr", [128, 8, 128], F32)
            ig = S.sbuf("B_ig", [128, 8, 128], F32)
            mu = S.sbuf("B_mu", [128, 8, 128], F32)
            hh = S.sbuf("B_hh", [128, 8, 128], F32)
            ps_tr = S.psum("B_ps_tr", [128, 8, 128], BF16)
            ps_xb = [S.psum("B_ps_xb%d" % i, [128, 4, 128], F32) for i in range(2)]
            ps_g = [S.psum("B_ps_g%d" % i, [128, 4, 128], F32) for i in range(2)]
            ps_r = S.psum("B_ps_r", [128, 4, 128], F32)
            ps_i = S.psum("B_ps_i", [128, 4, 128], F32)
            ps_ss = S.psum("B_ps_ss", [128, 512], F32)

            for i in range(2):
                S.memset("vector", hist[i][:], 0.0)
            S.memset("vector", hstate[:], 0.0)
            S.act(tmpl[:], pc("lam"), AF.Exp, scale=-1.0)
            S.act(tmpl[:], tmpl[:], AF.Ln, bias=one_t[:, 0:1])
            S.ts("vector", c8ls[:], tmpl[:], -8.0, None, ALU.mult)
            cwv = pc("cw").rearrange("p (c k) -> p c k", k=4)

            def bc(ap2, n):
                return ap2.unsqueeze(2).to_broadcast([128, ap2.shape[1], n])

            def front(t):
                tok0, ntok = TILES[t]
                hc = hist[t % 2]
                S.load("sync", xt[:ntok, :], h0_d[tok0:tok0 + ntok, :])
                norm_T("B", xt, ntok, junk, ss, rs, rstd, xn, ps_tr, hnT, "gmix")
                if t > 0:
                    pt = TILES[t - 1][1]
                    S.copy("vector", hc[:, :, 0:3], hist[(t - 1) % 2][:, :, pt:pt + 3])
                for b in range(2):
                    for j in range(4):
                        c = b * 4 + j
                        for k in range(16):
                            S.mm(ps_xb[b][:, j, :ntok], WL[k][:, c * 128:(c + 1) * 128], hnT[:, k, :ntok],
                                 start=(k == 0), stop=(k == 15))
                    S.tt("vector", hc[:, 4 * b:4 * b + 4, 3:3 + ntok], ps_xb[b][:, :, :ntok],
                         bc(pc("bxb", 4 * b, 4 * b + 4), ntok), ALU.add)
                gg = ggs[t % 2]
                for b in range(2):
                    for j in range(4):
                        c = b * 4 + j
                        for k in range(16):
                            S.mm(ps_g[b][:, j, :ntok], WL[k][:, 1024 + c * 128:1024 + (c + 1) * 128], hnT[:, k, :ntok],
                                 start=(k == 0), stop=(k == 15))
                    S.tt("vector", gg[:, 4 * b:4 * b + 4, :ntok], ps_g[b][:, :, :ntok],
                         bc(pc("bgate", 4 * b, 4 * b + 4), ntok), ALU.add)
                S.act(gg[:, :, :ntok], gg[:, :, :ntok], AF.Gelu_apprx_tanh)

            def back(t):
                tok0, ntok = TILES[t]
                hc = hist[t % 2]
                gg = ggs[t % 2]
                n = ntok
                S.tt("vector", xc[:, :, :n], hc[:, :, 3:3 + n], bc(cwv[:, :, 3], n), ALU.mult)
                S.tt("vector", xc[:, :, :n], xc[:, :, :n], bc(pc("cb"), n), ALU.add)
                for tap in range(3):
                    S.tt("vector", tmpw[:, :, :n], hc[:, :, tap:tap + n], bc(cwv[:, :, tap], n), ALU.mult)
                    S.tt("vector", xc[:, :, :n], xc[:, :, :n], tmpw[:, :, :n], ALU.add)
                for b in range(2):
                    for j in range(4):
                        c = b * 4 + j
                        S.mm(ps_r[:, j, :n], wri[:, c, :], xc[:, c, :n], start=True, stop=True)
                    S.tt("vector", rr[:, 4 * b:4 * b + 4, :n], ps_r[:, :, :n], bc(pc("br", 4 * b, 4 * b + 4), n), ALU.add)
                    for j in range(4):
                        c = b * 4 + j
                        S.mm(ps_i[:, j, :n], wri[:, 8 + c, :], xc[:, c, :n], start=True, stop=True)
                    S.tt("vector", ig[:, 4 * b:4 * b + 4, :n], ps_i[:, :, :n], bc(pc("bi", 4 * b, 4 * b + 4), n), ALU.add)
                S.act(rr[:, :, :n], rr[:, :, :n], AF.Sigmoid)
                S.act(ig[:, :, :n], ig[:, :, :n], AF.Sigmoid)
                S.tt("vector", rr[:, :, :n], rr[:, :, :n], bc(c8ls[:, :], n), ALU.mult)
                S.act(rr[:, :, :n], rr[:, :, :n], AF.Exp)
                S.act(mu[:, :, :n], rr[:, :, :n], AF.Square)
                S.act(mu[:, :, :n], mu[:, :, :n], AF.Sqrt, scale=-1.0, bias=one_t[:, 0:1])
                S.tt("vector", ig[:, :, :n], ig[:, :, :n], xc[:, :, :n], ALU.mult)
                S.tt("vector", ig[:, :, :n], ig[:, :, :n], mu[:, :, :n], ALU.mult)
                for c in range(8):
                    S.scan(hh[:, c, :n], rr[:, c, :n], ig[:, c, :n], hstate[:, c:c + 1], ALU.mult, ALU.add)
                S.copy("vector", hstate[:, :], hh[:, :, n - 1])
                S.tt("vector", hh[:, :, :n], hh[:, :, :n], gg[:, :, :n], ALU.mult)
                S.tt("vector", tmpw[:, :, :n], hh[:, :, :n], hh[:, :, :n], ALU.mult)
                S.tt("vector", mixr[:, :, tok0:tok0 + n], hh[:, :, :n], bc(pc("gr"), n), ALU.mult)
                for i in range(8):
                    S.mm(ps_ss[:n, 0:2], tmpw[:, i, :n], ones_f[:, 0:2], start=(i == 0), stop=(i == 7))
                S.act(sst[:n, :], ps_ss[:n, 0:1], AF.Sqrt, scale=1.0 / 1024, bias=eps_t[:n, :])
                S.recip(rstd_r[:n, t:t + 1], sst[:n, :])

            front(0)
            for t in range(NT):
                if t + 1 < NT:
                    front(t + 1)
                back(t)
        S.barrier()
        if DEBUG:
            S.store("sync", dbg["mixa"], mixa[:])
            S.store("sync", dbg["mixr"], mixr[:])
            S.store("sync", dbg["rstd"][:, 0, :], rstd_a[:])
            S.store("sync", dbg["rstd"][:, 1, :], rstd_r[:])

        with ExitStack() as ph:
            S.stack = ph
            WO = load_weights("O", wo_d, 2048)
            xts = [S.sbuf("O_xt%d" % i, [128, D], F32) for i in range(2)]
            h1s = [S.sbuf("O_h1%d" % i, [128, D], F32) for i in range(2)]
            psA = [S.psum("O_psA%d" % i, [128, 512], F32) for i in range(2)]
            psR = [S.psum("O_psR%d" % i, [128, 512], F32) for i in range(2)]
            for t in range(1, NT):
                tok0, ntok = TILES[t]
                xt = xts[t % 2]
                h1 = h1s[t % 2]
                S.load("sync", xt[:], h0_d[tok0:tok0 + 128, :])
                for n in range(4):
                    cs = slice(n * 512, (n + 1) * 512)
                    pa = psA[n % 2]
                    pr_ = psR[n % 2]
                    for i in range(8):
                        S.mm(pa[:], mixa[:, i, tok0:tok0 + 128], WO[i][:, cs], start=(i == 0), stop=(i == 7))
                    for i in range(8):
                        S.mm(pr_[:], mixr[:, i, tok0:tok0 + 128], WO[8 + i][:, cs], start=(i == 0), stop=(i == 7))
                    S.stt(h1[:, cs], pa[:], rstd_a[:, t:t + 1], xt[:, cs], ALU.mult, ALU.add)
                    S.stt(h1[:, cs], pr_[:], rstd_r[:, t:t + 1], h1[:, cs], ALU.mult, ALU.add)
                S.store("sync", h1_d[tok0 - NMETA:tok0 - NMETA + 128, :], h1[:], wkey="h1scr%d" % t)
        S.barrier()
        mid.close()

        with ExitStack() as ph:
            S.stack = ph
            skT = S.sbuf("C_skT", [128, 16, 128], BF16)
            WQ = load_weights("Q", wq_d, 2048, extra=[(skT, skT_d, [128, 16, 128])])
            gffn = S.sbuf("C_gffn", [128, D], F32)
            gfin = S.sbuf("C_gfin", [128, D], F32)
            S.load("sync", gffn[:], gffn_d)
            S.load("sync", gfin[:], gfin_d)
            h1s = [S.sbuf("C_h1_%d" % i, [128, D], F32) for i in range(2)]
            hn2s = [S.sbuf("C_hn2_%d" % i, [128, D], F32) for i in range(2)]
            xn = S.sbuf("C_xn", [128, D], BF16)
            junkA = xn
            ss = S.sbuf("C_ss", [128, 1], F32)
            rs = S.sbuf("C_rs", [128, 1], F32)
            rstd = S.sbuf("C_rstd", [128, 1], F32)
            ss2 = S.sbuf("C_ss2", [128, 1], F32)
            rs2 = S.sbuf("C_rs2", [128, 1], F32)
            rstd2 = S.sbuf("C_rstd2", [128, 1], F32)
            hnT = S.sbuf("C_hnT", [128, 16, 128], BF16)
            qT = S.sbuf("C_qT", [128, 16, 128], BF16)
            s_sb = S.sbuf("C_s", [128, 16, 128], F32)
            swork = S.sbuf("C_swork", [128, 128], F32)
            topv = S.sbuf("C_topv", [128, 16, 16], F32)
            topi = S.sbuf("C_topi", [128, 16, 16], U32)
            topif = S.sbuf("C_topif", [128, 16, 16], F32)
            cand = S.sbuf("C_cand", [128, 16, 16], F32)
            cwork = S.sbuf("C_cwork", [128, 16, 16], F32)
            bestv = S.sbuf("C_bestv", [128, 8, 16], F32)
            besti = S.sbuf("C_besti", [128, 8, 16], U32)
            bestif = S.sbuf("C_bestif", [128, 8, 16], F32)
            k1f = S.sbuf("C_k1f", [128, 8, 16], F32)
            k2f = S.sbuf("C_k2f", [128, 8, 16], F32)
            ohv = s_sb[:].rearrange("p (h x) (y k) -> p h (x y) k", x=2, k=16)
            i1f = S.sbuf("C_i1f", [128, 8, 16], F32)
            i2f = S.sbuf("C_i2f", [128, 8, 16], F32)
            ef = S.sbuf("C_ef", [128, 128], F32)
            eis = [S.sbuf("C_ei%d" % i, [128, 128], I32) for i in range(2)]
            gex = S.sbuf("C_gex", [128, 8, 16], F32)
            gsum = S.sbuf("C_gsum", [128, 8], F32)
            gws = [S.sbuf("C_gw%d" % i, [128, 8, 16], F32) for i in range(2)]
            NRG = 8
            avs = [S.sbuf("C_av%d" % i, [128, 1], F32) for i in range(NRG)]
            gas = [S.sbuf("C_ga%d" % i, [128, 1], F32) for i in range(NRG)]
            w2s = [S.sbuf("C_w2%d" % i, [128, 1], F32) for i in range(NRG)]
            NGB, NDG = 6, 6
            gbuf = [S.sbuf("C_gb%d" % i, [128, 2 * D], BF16) for i in range(NGB)]
            uvflat = uvtb_d.rearrange("e two d -> e (two d)")
            TB_ALL = TB_KEYS["u"] + TB_KEYS["v"]
            gctr = [0]

            def next_gbuf():
                b = gbuf[gctr[0] % NGB]
                gctr[0] += 1
                return b
            dgs = [S.sbuf("C_dg%d" % i, [128, 128], BF16) for i in range(NDG)]
            ps_tr = S.psum("C_ps_tr", [128, 8, 128], BF16)
            ps_q = S.psum("C_ps_q", [128, 4, 128], F32)
            ps_sc = S.psum("C_ps_sc", [128, 4, 128], F32)
            ps_acc = [S.psum("C_ps_acc%d" % n, [128, 512], F32) for n in range(4)]
            iota = pr("iota")
            last_t = n_tiles_c

            def prologue_steps(t):
                tok0, ntok = TILES[t]
                r0 = tok0 - NMETA
                h1 = h1s[t % 2]
                hn2 = hn2s[t % 2]
                ei = eis[t % 2]
                gw = gws[t % 2]
                steps = []

                def a0():
                    S.load("sync", h1[:], h1_d[r0:r0 + 128, :], reads=["h1scr%d" % t])
                    S.act(junkA[:], h1[:], AF.Square, accum_out=ss[:, 0:1])
                    S.act(rs[:], ss[:], AF.Sqrt, scale=1.0 / D, bias=eps_t[:])
                steps.append(a0)

                def a1():
                    S.recip(rstd[:], rs[:])
                    S.stt(hn2[:], h1[:], rstd[:, 0:1], gffn[:], ALU.mult, ALU.mult)
                    S.copy("scalar", xn[:], hn2[:])
                steps.append(a1)
                for half in range(2):
                    def a2(half=half):
                        for kk in range(8):
                            k = half * 8 + kk
                            S.tr(ps_tr[:, kk, :], xn[:, k * 128:(k + 1) * 128], identb[:])
                    steps.append(a2)

                    def a3(half=half):
                        S.copy("vector", hnT[:, half * 8:(half + 1) * 8, :], ps_tr[:])
                    steps.append(a3)
                for grp in range(4):
                    def b0(grp=grp):
                        for j in range(4):
                            hp = grp * 4 + j
                            for k in range(16):
                                S.mm(ps_q[:, j, :], WQ[k][:, hp * 128:(hp + 1) * 128], hnT[:, k, :],
                                     start=(k == 0), stop=(k == 15))
                    steps.append(b0)

                    def b1(grp=grp):
                        S.copy("scalar", qT[:, grp * 4:grp * 4 + 4, :], ps_q[:])
                    steps.append(b1)
                for grp in range(4):
                    def c0(grp=grp):
                        for j in range(4):
                            hp = grp * 4 + j
                            S.mm(ps_sc[:, j, :], qT[:, hp, :], skT[:, hp, :], start=True, stop=True)
                    steps.append(c0)

                    def c1(grp=grp):
                        S.copy("scalar", s_sb[:, grp * 4:grp * 4 + 4, :], ps_sc[:])
                    steps.append(c1)
                for hp in range(16):
                    def d0(hp=hp):
                        S.vmax(topv[:, hp, 0:8], s_sb[:, hp, :])
                        S.vmax_index(topi[:, hp, 0:8], topv[:, hp, 0:8], s_sb[:, hp, :])
                        S.vmatch_replace(swork[:], topv[:, hp, 0:8], s_sb[:, hp, :], -1e30)
                        S.vmax(topv[:, hp, 8:16], swork[:])
                        S.vmax_index(topi[:, hp, 8:16], topv[:, hp, 8:16], swork[:])
                    steps.append(d0)

                def d1():
                    S.copy("vector", topif[:], topi[:])
                steps.append(d1)
                for h in range(8):
                    def e0(h=h):
                        S.tt("vector", cand[:], topv[:, 2 * h, :].unsqueeze(2).to_broadcast([128, 16, 16]),
                             topv[:, 2 * h + 1, :].unsqueeze(1).to_broadcast([128, 16, 16]), ALU.add)
                        cflat = cand[:].rearrange("p a b -> p (a b)")
                        wflat = cwork[:].rearrange("p a b -> p (a b)")
                        S.vmax(bestv[:, h, 0:8], cflat)
                        S.vmax_index(besti[:, h, 0:8], bestv[:, h, 0:8], cflat)
                        S.vmatch_replace(wflat, bestv[:, h, 0:8], cflat, -1e30)
                        S.vmax(bestv[:, h, 8:16], wflat)
                        S.vmax_index(besti[:, h, 8:16], bestv[:, h, 8:16], wflat)
                    steps.append(e0)

                def f0():
                    S.copy("vector", bestif[:], besti[:])
                    S.tt("vector", ohv, bestif[:].unsqueeze(3).to_broadcast([128, 8, 16, 16]),
                         pr("thr").unsqueeze(1).unsqueeze(1).to_broadcast([128, 8, 16, 16]), ALU.is_ge)
                    S.reduce(k1f[:], ohv, AX.X, ALU.add)
                    S.stt(k2f[:], k1f[:], -16.0, bestif[:], ALU.mult, ALU.add)
                steps.append(f0)
                tview = topif[:].rearrange("p (h two) k -> p h two k", two=2)
                for (kf, half_, dst) in ((k1f, 0, i1f), (k2f, 1, i2f)):
                    def f1(kf=kf, half_=half_, dst=dst):
                        S.tt("vector", ohv, iota.unsqueeze(1).unsqueeze(1).to_broadcast([128, 8, 16, 16]),
                             kf[:].unsqueeze(3).to_broadcast([128, 8, 16, 16]), ALU.is_equal)
                        S.tt("vector", ohv, ohv,
                             tview[:, :, half_, :].unsqueeze(2).to_broadcast([128, 8, 16, 16]), ALU.mult)
                        S.reduce(dst[:], ohv, AX.X, ALU.add)
                    steps.append(f1)

                def f2():
                    S.stt(ef[:], i1f[:].rearrange("p h k -> p (h k)"), 128.0,
                          i2f[:].rearrange("p h k -> p (h k)"), ALU.mult, ALU.add)
                    S.copy("vector", ei[:], ef[:])
                    S.tt("vector", gex[:], bestv[:], bestv[:, :, 0:1].to_broadcast([128, 8, 16]), ALU.subtract)
                    S.act(gex[:], gex[:], AF.Exp)
                    S.reduce(gsum[:], gex[:], AX.X, ALU.add)
                    S.recip(gsum[:], gsum[:])
                    S.tt("vector", gw[:], gex[:], gsum[:].unsqueeze(2).to_broadcast([128, 8, 16]), ALU.mult)
                steps.append(f2)
                return steps

            def slot(t, sl):
                gb = next_gbuf()
                av, ga, w2, dg = avs[sl % NRG], gas[sl % NRG], w2s[sl % NRG], dgs[sl % NDG]
                S.gather(gb[:], uvflat, eis[t % 2][:, sl:sl + 1], extra_reads=TB_ALL)
                S.stt(gb[:, 0:D], gb[:, 0:D], 1.0, hn2s[t % 2][:], ALU.mult, ALU.mult, accum_out=av[:, 0:1])
                S.act(ga[:], av[:], AF.Gelu_apprx_tanh)
                S.act(w2[:], ga[:], AF.Copy, scale=gws[t % 2][:].rearrange("p h k -> p (h k)")[:, sl:sl + 1])
                S.act(dg[:], identf[:], AF.Copy, scale=w2[:, 0:1])
                for n in range(4):
                    S.mm(ps_acc[n][:], dg[:], gb[:, D + n * 512:D + (n + 1) * 512], start=(sl == 0), stop=(sl == 127))

            def F(t):
                tok0, ntok = TILES[t]
                r0 = tok0 - NMETA
                h1 = h1s[t % 2]
                for n in range(4):
                    cs = slice(n * 512, (n + 1) * 512)
                    S.tt("vector", h1[:, cs], ps_acc[n][:], h1[:, cs], ALU.add)
                S.act(junkA[:], h1[:], AF.Square, accum_out=ss2[:, 0:1])
                S.act(rs2[:], ss2[:], AF.Sqrt, scale=1.0 / D, bias=eps_t[:])
                S.recip(rstd2[:], rs2[:])
                S.stt(h1[:], h1[:], rstd2[:, 0:1], gfin[:], ALU.mult, ALU.mult)
                S.store("sync", y_d[r0:r0 + 128, :], h1[:])

            for st_ in prologue_steps(1):
                st_()
            for t in range(1, last_t + 1):
                steps = prologue_steps(t + 1) if t + 1 <= last_t else []
                sched = {}
                for i, st_ in enumerate(steps):
                    sched.setdefault(min(127, (i * 100) // max(1, len(steps))), []).append(st_)
                for sl in range(128):
                    slot(t, sl)
                    for st_ in sched.get(sl, []):
                        st_()
                F(t)
        S.barrier()
        S.stack = gst
        S.emit()
    return nc


def _prep_shared(inp):
    f = np.float32
    w_in = np.asarray(inp["w_in"][0], f)
    b_in = np.asarray(inp["b_in"][0], f)
    q_w, q_b = w_in[:, 0:1024], b_in[0:1024]
    k0_w, k1_w = w_in[:, 1024:1088], w_in[:, 1088:1152]
    v0_w, v1_w = w_in[:, 1152:1216], w_in[:, 1216:1280]
    k0_b, k1_b = b_in[1024:1088], b_in[1088:1152]
    v0_b, v1_b = b_in[1152:1216], b_in[1216:1280]
    wa = np.ascontiguousarray(np.concatenate([q_w, k0_w, k0_w, k1_w, k1_w, v0_w, v0_w, v1_w, v1_w], axis=1))
    wl = np.ascontiguousarray(w_in[:, 1280:3328])
    pcol = np.zeros((128, NPCOL), f)

    def put(name, arr):
        o, w = PCOL[name]
        assert arr.shape == (128, w), (name, arr.shape)
        pcol[:, o:o + w] = arr

    def colmajor(v):
        return np.ascontiguousarray(np.asarray(v, f).reshape(-1, 128).T)

    put("gmix", colmajor(inp["norm_mix_g"][0]))
    put("bq", colmajor(q_b))
    put("bk", np.stack([np.concatenate([k0_b, k0_b]), np.concatenate([k1_b, k1_b])], axis=1))
    put("bxb", colmajor(b_in[1280:2304]))
    put("bgate", colmajor(b_in[2304:3328]))
    cw = np.asarray(inp["conv_w"][0], f)
    cwl = cw.T.reshape(8, 128, 4).transpose(1, 0, 2).reshape(128, 32)
    put("cw", np.ascontiguousarray(cwl))
    put("cb", colmajor(inp["conv_b"][0]))
    put("br", colmajor(np.asarray(inp["b_r"][0], f).reshape(-1)))
    put("bi", colmajor(np.asarray(inp["b_i"][0], f).reshape(-1)))
    put("lam", colmajor(inp["lru_lambda"][0]))
    put("gr", colmajor(inp["gn_lru_g"][0]))
    put("ga", colmajor(inp["gn_attn_g"][0]))
    prow = np.zeros((128, NPROW), f)
    o, w = PROW["bv"]
    prow[:, o:o + w] = np.concatenate([v0_b, v0_b, v1_b, v1_b])[None, :]
    sinks = np.asarray(inp["sinks"][0], f)
    sq = np.zeros(16, f)
    for g in range(2):
        for par in range(2):
            for j in range(4):
                sq[(g * 2 + par) * 4 + j] = sinks[8 * g + 2 * j + par]
    o, w = PROW["sink"]
    prow[:, o:o + w] = sq[None, :]
    o, w = PROW["iota"]
    prow[:, o:o + w] = np.arange(16, dtype=f)[None, :]
    o, w = PROW["thr"]
    prow[:, o:o + w] = np.arange(16, 272, 16, dtype=f)[None, :]
    w_r = np.asarray(inp["w_r"][0], f)
    w_i = np.asarray(inp["w_i"][0], f)
    wri = np.zeros((128, 16, 128), f)
    for c in range(8):
        for half in range(2):
            sl = slice(half * 64, half * 64 + 64)
            wri[sl, c, sl] = w_r[2 * c + half]
            wri[sl, 8 + c, sl] = w_i[2 * c + half]
    sk = np.asarray(inp["peer_sub_keys"][0], f)
    skT = np.ascontiguousarray(sk.reshape(16, 128, 128).transpose(2, 0, 1))
    masks = np.zeros((128, 2, 128), f)
    jj = np.arange(128)[:, None]
    ii = np.arange(128)[None, :]
    masks[:, 0, :] = (jj > ii)
    masks[:, 1, :] = (jj <= ii)
    shared = {
        "wa": wa, "wl": wl,
        "wo": np.ascontiguousarray(np.asarray(inp["w_out"][0], f)),
        "wq": np.ascontiguousarray(np.asarray(inp["peer_wq"][0], f)),
        "wri": wri, "skT": skT, "pcol": pcol, "prow": prow,
        "gffn": np.ascontiguousarray(np.broadcast_to(np.asarray(inp["norm_ffn_g"][0], f)[None, :], (128, D))),
        "gfin": np.ascontiguousarray(np.broadcast_to(np.asarray(inp["final_norm_g"], f)[None, :], (128, D))),
        "ident": np.eye(128, dtype=f), "masks": masks,
        "utab": np.ascontiguousarray(np.asarray(inp["peer_u"][0], f)),
        "vtab": np.ascontiguousarray(np.asarray(inp["peer_v"][0], f)),
    }
    return shared


_NC_CACHE = {}


def kernel(**inputs):
    x = np.asarray(inputs["x"], np.float32)
    meta = np.asarray(inputs["meta_tokens"], np.float32)
    B = x.shape[0]
    shared = _prep_shared(inputs)
    if "nc" not in _NC_CACHE:
        _NC_CACHE["nc"] = build_program()
    nc = _NC_CACHE["nc"]
    in_maps = []
    for b in range(B):
        m = dict(shared)
        m["h0"] = np.ascontiguousarray(np.concatenate([meta, x[b]], axis=0))
        in_maps.append(m)
    res = run_bass_kernel_spmd(nc, in_maps, core_ids=list(range(B)))
    out = np.stack([np.asarray(r["y"], np.float32) for r in res.results], axis=0)
    return out
```
